# Optimizing a Trainium2 kernel written in Bass

```python
import math
import jax
import jax.numpy as jnp
from jax import lax
import numpy as np

D_MODEL = 1024
BATCH = 16
SEQ = 4096
DEPTH = 1

N_META = 16
BLOCK = 128
PAD = BLOCK - N_META
NEG = -1e30
LN_EPS = 1e-5

ATT_HEADS = 4
ATT_QK_DIM = 64
ATT_V_DIM = 2 * ATT_QK_DIM
ATT_WIDTH = ATT_HEADS * ATT_V_DIM
ROPE_THETA = 10000.0

ML_HEADS = 4
ML_DH = 128
ML_WIDTH = ML_HEADS * ML_DH
CONV_K = 4

N_EXPERTS = 32
TOP_K = 4
D_EXPERT = D_MODEL
SWIGLU_LIMIT = 7.0
SWIGLU_ALPHA = 1.702
MOE_BLOCK = 512

DN_ALPHA = (2 * DEPTH) ** 0.25
DN_BETA = (8 * DEPTH) ** -0.25

IN_SPLITS = (ATT_HEADS * 2 * ATT_QK_DIM, ATT_HEADS * 2 * ATT_QK_DIM, ATT_WIDTH,
             2 * ML_WIDTH, ML_WIDTH, ML_WIDTH, 2 * ML_HEADS, D_MODEL, D_MODEL)
N_IN = sum(IN_SPLITS)

kernel_name = 'hybrid_diffattn_mlstm_moe_block'


def layer_norm(x, g, b):
    xf = x.astype(jnp.float32)
    mu = jnp.mean(xf, axis=-1, keepdims=True)
    var = jnp.mean(jnp.square(xf - mu), axis=-1, keepdims=True)
    y = (xf - mu) * lax.rsqrt(var + LN_EPS) * g.astype(jnp.float32) + b.astype(jnp.float32)
    return y.astype(x.dtype)


def rms_norm(x, g):
    xf = x.astype(jnp.float32)
    y = xf * lax.rsqrt(jnp.mean(jnp.square(xf), axis=-1, keepdims=True) + LN_EPS) * g.astype(jnp.float32)
    return y.astype(x.dtype)


def rotary(x, pos):
    half = x.shape[-1] // 2
    inv_freq = ROPE_THETA ** (-jnp.arange(half, dtype=jnp.float32) / half)
    ang = pos.astype(jnp.float32)[:, None] * inv_freq[None, :]
    cos = jnp.cos(ang)[:, None, :]
    sin = jnp.sin(ang)[:, None, :]
    xf = x.astype(jnp.float32)
    x1, x2 = xf[..., :half], xf[..., half:]
    return jnp.concatenate([x1 * cos - x2 * sin, x2 * cos + x1 * sin], axis=-1).astype(x.dtype)


def causal_conv(x, w, b):
    y = lax.conv_general_dilated(x, w[:, None, :], window_strides=(1,), padding=((CONV_K - 1, 0),),
                                 dimension_numbers=('NWC', 'WIO', 'NWC'),
                                 feature_group_count=x.shape[-1])
    return y + b


def diff_attention(q, k, v, lam, lambda_init, norm_g):
    B, T = q.shape[0], q.shape[1]
    Tp = T + PAD
    def pad(a):
        return jnp.pad(a, ((0, 0), (PAD, 0)) + ((0, 0),) * (a.ndim - 2))
    qh = pad(q).transpose(0, 2, 3, 1, 4)
    kh = pad(k).transpose(0, 2, 3, 1, 4)
    vh = pad(v).transpose(0, 2, 1, 3)
    key_idx = jnp.arange(Tp)
    scale = ATT_QK_DIM ** -0.5

    def one_block(start):
        qb = lax.dynamic_slice_in_dim(qh, start, BLOCK, axis=3)
        s = jnp.einsum('bhmqd,bhmkd->bhmqk', qb, kh).astype(jnp.float32) * scale
        q_idx = start + jnp.arange(BLOCK)
        mask = (key_idx[None, :] <= q_idx[:, None]) & (key_idx[None, :] >= PAD)
        p = jax.nn.softmax(jnp.where(mask, s, NEG), axis=-1)
        a = p[:, :, 0] - lam * p[:, :, 1]
        return jnp.einsum('bhqk,bhke->bqhe', a.astype(vh.dtype), vh)

    starts = jnp.arange(Tp // BLOCK) * BLOCK
    o = lax.map(one_block, starts)
    o = o.transpose(1, 0, 2, 3, 4).reshape(B, Tp, ATT_HEADS, ATT_V_DIM)[:, PAD:]
    o = rms_norm(o, norm_g) * (1.0 - lambda_init)
    return o.reshape(B, T, ATT_WIDTH)


def mlstm_chunk(carry, inp):
    C, n, m = carry
    q, k, v, ig, lf = inp
    causal = jnp.tril(jnp.ones((BLOCK, BLOCK), dtype=bool))
    b = jnp.cumsum(lf, axis=-1)
    dlog = jnp.where(causal, b[..., :, None] - b[..., None, :] + ig[..., None, :], NEG)
    inter = b + m[..., None]
    m_s = jnp.maximum(inter, jnp.max(dlog, axis=-1))
    w_intra = jnp.exp(dlog - m_s[..., None])
    w_inter = jnp.exp(inter - m_s)
    s = jnp.einsum('bhsd,bhrd->bhsr', q, k) * w_intra
    num = w_inter[..., None] * jnp.einsum('bhed,bhsd->bhse', C, q) + jnp.einsum('bhsr,bhre->bhse', s, v)
    den = w_inter * jnp.einsum('bhd,bhsd->bhs', n, q) + jnp.sum(s, axis=-1)
    h = num / jnp.maximum(jnp.abs(den), jnp.exp(-m_s))[..., None]
    b_last = b[..., -1]
    upd = b_last[..., None] - b + ig
    m_new = jnp.maximum(b_last + m, jnp.max(upd, axis=-1))
    w_old = jnp.exp(b_last + m - m_new)
    w_r = jnp.exp(upd - m_new[..., None])
    C_new = w_old[..., None, None] * C + jnp.einsum('bhre,bhrd->bhed', v * w_r[..., None], k)
    n_new = w_old[..., None] * n + jnp.einsum('bhr,bhrd->bhd', w_r, k)
    return (C_new, n_new, m_new), h


def mlstm(q, k, v, ig, lf):
    B, T = q.shape[0], q.shape[1]
    Tp = T + PAD
    nc = Tp // BLOCK
    def chunks(a, fill):
        a = jnp.pad(a.astype(jnp.float32), ((0, 0), (PAD, 0)) + ((0, 0),) * (a.ndim - 2),
                    constant_values=fill)
        a = a.reshape((B, nc, BLOCK) + a.shape[2:])
        return jnp.swapaxes(jnp.swapaxes(a, 0, 1), 2, 3)
    qc = chunks(q, 0.0)
    kc = chunks(k * (ML_DH ** -0.5), 0.0)
    vc = chunks(v, 0.0)
    igc = chunks(ig, NEG)
    lfc = chunks(lf, 0.0)
    init = (jnp.zeros((B, ML_HEADS, ML_DH, ML_DH), jnp.float32),
            jnp.zeros((B, ML_HEADS, ML_DH), jnp.float32),
            jnp.zeros((B, ML_HEADS), jnp.float32))
    _, h = lax.scan(mlstm_chunk, init, (qc, kc, vc, igc, lfc))
    h = jnp.swapaxes(jnp.swapaxes(h, 2, 3), 0, 1).reshape(B, Tp, ML_HEADS, ML_DH)[:, PAD:]
    return h


def hybrid_mixer(h, pos, lambda_init, w_in, conv_w, conv_b, gate_bias, lam_q1, lam_k1, lam_q2, lam_k2,
                 att_norm_g, ml_norm_g, w_att_out, w_ml_out, w_o):
    B, T, _ = h.shape
    z = h @ w_in
    aq, ak, av, mqk, mv, mo, mgate, ga, gm = jnp.split(z, np.cumsum(IN_SPLITS)[:-1].tolist(), axis=-1)
    aq = rotary(aq.reshape(B, T, 2 * ATT_HEADS, ATT_QK_DIM), pos).reshape(B, T, ATT_HEADS, 2, ATT_QK_DIM)
    ak = rotary(ak.reshape(B, T, 2 * ATT_HEADS, ATT_QK_DIM), pos).reshape(B, T, ATT_HEADS, 2, ATT_QK_DIM)
    lam = (jnp.exp(jnp.sum(lam_q1.astype(jnp.float32) * lam_k1.astype(jnp.float32)))
           - jnp.exp(jnp.sum(lam_q2.astype(jnp.float32) * lam_k2.astype(jnp.float32))) + lambda_init)
    y_att = diff_attention(aq, ak, av.reshape(B, T, ATT_HEADS, ATT_V_DIM), lam, lambda_init, att_norm_g)
    mqk = jax.nn.silu(causal_conv(mqk, conv_w, conv_b))
    mq, mk = jnp.split(mqk, 2, axis=-1)
    gates = mgate.astype(jnp.float32) + gate_bias.astype(jnp.float32)
    ig = gates[..., :ML_HEADS]
    lf = jax.nn.log_sigmoid(gates[..., ML_HEADS:])
    hm = mlstm(mq.reshape(B, T, ML_HEADS, ML_DH), mk.reshape(B, T, ML_HEADS, ML_DH),
               mv.reshape(B, T, ML_HEADS, ML_DH), ig, lf)
    hm = rms_norm(hm, ml_norm_g.reshape(ML_HEADS, ML_DH)).astype(h.dtype)
    y_ml = jax.nn.sigmoid(mo) * hm.reshape(B, T, ML_WIDTH)
    merged = jax.nn.sigmoid(ga) * (y_att @ w_att_out) + jax.nn.sigmoid(gm) * (y_ml @ w_ml_out)
    return merged @ w_o


def moe(x, w_router, b_router, w_gu, b_gu, w_down, b_down):
    N, D = x.shape
    logits = x.astype(jnp.float32) @ w_router.astype(jnp.float32) + b_router.astype(jnp.float32)
    top_val, top_idx = lax.top_k(logits, TOP_K)
    gate = jax.nn.softmax(top_val, axis=-1)
    M = N * TOP_K
    e_flat = top_idx.reshape(M).astype(jnp.int32)
    t_flat = jnp.repeat(jnp.arange(N, dtype=jnp.int32), TOP_K)
    order = jnp.argsort(e_flat)
    e_s, t_s, g_s = e_flat[order], t_flat[order], gate.reshape(M)[order]
    counts = jnp.bincount(e_flat, length=N_EXPERTS)
    starts = jnp.cumsum(counts) - counts
    padded = (counts + MOE_BLOCK - 1) // MOE_BLOCK * MOE_BLOCK
    pends = jnp.cumsum(padded)
    pstarts = pends - padded
    dest = pstarts[e_s] + jnp.arange(M, dtype=jnp.int32) - starts[e_s]
    nb = -(-(M + N_EXPERTS * (MOE_BLOCK - 1)) // MOE_BLOCK)
    tok_buf = jnp.full((nb * MOE_BLOCK,), N, jnp.int32).at[dest].set(t_s).reshape(nb, MOE_BLOCK)
    gate_buf = jnp.zeros((nb * MOE_BLOCK,), jnp.float32).at[dest].set(g_s).reshape(nb, MOE_BLOCK)
    blk_exp = jnp.minimum(jnp.searchsorted(pends, jnp.arange(nb, dtype=jnp.int32) * MOE_BLOCK, side='right'),
                          N_EXPERTS - 1)
    x_ext = jnp.concatenate([x, jnp.zeros((1, D), x.dtype)], axis=0)

    def expert_block(acc, inp):
        toks, g, e = inp
        hid = x_ext[toks] @ w_gu[e] + b_gu[e]
        glu = jnp.minimum(hid[:, ::2], SWIGLU_LIMIT)
        lin = jnp.clip(hid[:, 1::2], -SWIGLU_LIMIT, SWIGLU_LIMIT)
        act = glu * jax.nn.sigmoid(SWIGLU_ALPHA * glu) * (lin + 1.0)
        y = (act @ w_down[e] + b_down[e]) * g.astype(x.dtype)[:, None]
        return acc.at[toks].add(y.astype(acc.dtype)), None

    acc, _ = lax.scan(expert_block, jnp.zeros((N + 1, D), x.dtype), (tok_buf, gate_buf, blk_exp))
    return acc[:N]


def setup_inputs(seed: int = 0) -> dict:
    key = jax.random.key(seed)
    ks = jax.random.split(key, 32)
    f32 = jnp.float32
    L, D, E, F = DEPTH, D_MODEL, N_EXPERTS, D_EXPERT

    def nrm(k, shape, scale):
        return jax.random.normal(k, shape, f32) * scale

    f_bias = jnp.linspace(3.0, 6.0, ML_HEADS, dtype=f32)[None, :] + nrm(ks[7], (L, ML_HEADS), 0.1)
    i_bias = nrm(ks[8], (L, ML_HEADS), 0.1)
    return {
        'x': nrm(ks[0], (BATCH, SEQ, D), 1.0),
        'meta': nrm(ks[1], (N_META, D), 1.0),
        'emb_ln_g': 1.0 + nrm(ks[2], (D,), 0.02),
        'emb_ln_b': nrm(ks[3], (D,), 0.02),
        'w_in': nrm(ks[4], (L, D, N_IN), D ** -0.5),
        'conv_w': nrm(ks[5], (L, CONV_K, 2 * ML_WIDTH), CONV_K ** -0.5),
        'conv_b': nrm(ks[6], (L, 2 * ML_WIDTH), 0.02),
        'gate_bias': jnp.concatenate([i_bias, f_bias], axis=-1),
        'lam_q1': nrm(ks[9], (L, ATT_QK_DIM), 0.1),
        'lam_k1': nrm(ks[10], (L, ATT_QK_DIM), 0.1),
        'lam_q2': nrm(ks[11], (L, ATT_QK_DIM), 0.1),
        'lam_k2': nrm(ks[12], (L, ATT_QK_DIM), 0.1),
        'att_norm_g': 1.0 + nrm(ks[13], (L, ATT_V_DIM), 0.02),
        'ml_norm_g': 1.0 + nrm(ks[14], (L, ML_WIDTH), 0.02),
        'w_att_out': nrm(ks[15], (L, ATT_WIDTH, D), ATT_WIDTH ** -0.5 * DN_BETA),
        'w_ml_out': nrm(ks[16], (L, ML_WIDTH, D), ML_WIDTH ** -0.5 * DN_BETA),
        'w_o': nrm(ks[17], (L, D, D), D ** -0.5 * DN_BETA),
        'ln1_g': 1.0 + nrm(ks[18], (L, D), 0.02),
        'ln1_b': nrm(ks[19], (L, D), 0.02),
        'w_router': nrm(ks[20], (L, D, E), D ** -0.5),
        'b_router': nrm(ks[21], (L, E), 0.01),
        'w_gu': nrm(ks[22], (L, E, D, 2 * F), D ** -0.5),
        'b_gu': nrm(ks[23], (L, E, 2 * F), 0.02),
        'w_down': nrm(ks[24], (L, E, F, D), F ** -0.5 * DN_BETA),
        'b_down': nrm(ks[25], (L, E, D), 0.02),
        'ln2_g': 1.0 + nrm(ks[26], (L, D), 0.02),
        'ln2_b': nrm(ks[27], (L, D), 0.02),
    }


def reference(x, meta, emb_ln_g, emb_ln_b, w_in, conv_w, conv_b, gate_bias, lam_q1, lam_k1, lam_q2, lam_k2,
              att_norm_g, ml_norm_g, w_att_out, w_ml_out, w_o, ln1_g, ln1_b, w_router, b_router,
              w_gu, b_gu, w_down, b_down, ln2_g, ln2_b):
    B = x.shape[0]
    h = jnp.concatenate([jnp.broadcast_to(meta[None], (B, N_META, D_MODEL)).astype(x.dtype), x], axis=1)
    h = layer_norm(h, emb_ln_g, emb_ln_b)
    T = h.shape[1]
    pos = jnp.arange(T)
    for l in range(DEPTH):
        lambda_init = 0.8 - 0.6 * math.exp(-0.3 * l)
        mix = hybrid_mixer(h, pos, lambda_init, w_in[l], conv_w[l], conv_b[l], gate_bias[l],
                           lam_q1[l], lam_k1[l], lam_q2[l], lam_k2[l], att_norm_g[l], ml_norm_g[l],
                           w_att_out[l], w_ml_out[l], w_o[l])
        h = layer_norm(DN_ALPHA * h + mix, ln1_g[l], ln1_b[l])
        ffn = moe(h.reshape(B * T, D_MODEL), w_router[l], b_router[l], w_gu[l], b_gu[l],
                  w_down[l], b_down[l]).reshape(B, T, D_MODEL)
        h = layer_norm(DN_ALPHA * h + ffn, ln2_g[l], ln2_b[l])
    return h[:, N_META:]
```

```python
import math
from contextlib import ExitStack
import numpy as np
import concourse.bass as bass
import concourse.mybir as mybir
from concourse.bass_utils import run_bass_kernel_spmd

F32 = mybir.dt.float32
F32R = mybir.dt.float32r
I32 = mybir.dt.int32
BF16 = mybir.dt.bfloat16
AF = mybir.ActivationFunctionType
ALU = mybir.AluOpType
AX = mybir.AxisListType

ENGS = ('pe', 'act', 'dve', 'pool', 'sp')
D = 1024
NIN = 5640
NEG = -1e30
EPS = 1e-5
ALPHA = 2.0 ** 0.25
LAMBDA_INIT = 0.8 - 0.6 * math.exp(0.0)


class Sched:
    def __init__(self, nc, stack, n_dma_sems=48):
        self.nc = nc
        self.ops = {e: [] for e in ENGS}
        self.sem = {e: stack.enter_context(nc.semaphore('s_' + e)) for e in ENGS}
        self.cnt = {e: 0 for e in ENGS}
        self.waited = {e: {} for e in ENGS}
        self.last_w = {}
        self.readers = {}
        self.dsem = [stack.enter_context(nc.semaphore('d%d' % i)) for i in range(n_dma_sems)]
        self.dcnt = [0] * n_dma_sems
        self.drr = 0
        self.drr_sw = 0

    def _deps(self, reads, writes):
        deps = set()
        for t in reads:
            if t in self.last_w:
                deps.add(self.last_w[t])
        for t in writes:
            if t in self.last_w:
                deps.add(self.last_w[t])
            for r in self.readers.get(t, ()):
                deps.add(r)
        return deps

    def _emit_waits(self, eng, deps, is_dma):
        best = {}
        for (k, v) in deps:
            if best.get(k, 0) < v:
                best[k] = v
        waits = []
        for k, v in best.items():
            if k == 'pe' and eng == 'pe' and not is_dma:
                continue
            if self.waited[eng].get(k, 0) >= v:
                continue
            self.waited[eng][k] = v
            waits.append((k, v))
        return waits

    def _semof(self, k):
        return self.sem[k] if isinstance(k, str) else self.dsem[k]

    def _record(self, me, reads, writes):
        for t in writes:
            self.last_w[t] = me
            self.readers[t] = []
        for t in reads:
            self.readers.setdefault(t, []).append(me)

    def barrier(self):
        deps = set((e, self.cnt[e]) for e in ENGS if self.cnt[e] > 0)
        deps |= set((i, v) for i, v in enumerate(self.dcnt) if v > 0)
        self.need_barrier = {e: set(deps) for e in ENGS}

    def _deps_b(self, eng, reads, writes):
        deps = self._deps(reads, writes)
        nb = getattr(self, 'need_barrier', {}).pop(eng, None)
        if nb:
            deps |= nb
        return deps

    def op(self, eng, fn, reads=(), writes=()):
        waits = self._emit_waits(eng, self._deps_b(eng, reads, writes), False)
        self.cnt[eng] += 1
        me = (eng, self.cnt[eng])
        self.ops[eng].append((waits, fn, (eng, 1)))
        self._record(me, reads, writes)
        return me

    def dma(self, eng, fn, reads=(), writes=()):
        deps = self._deps_b(eng, reads, writes)
        nsw = 16
        if eng == 'pool':
            si = self.drr_sw
            self.drr_sw = (self.drr_sw + 1) % nsw
        else:
            si = nsw + self.drr
            self.drr = (self.drr + 1) % (len(self.dsem) - nsw)
        if self.dcnt[si] > 0:
            deps.add((si, self.dcnt[si]))
        waits = self._emit_waits(eng, deps, True)
        self.dcnt[si] += 16
        me = (si, self.dcnt[si])
        self.ops[eng].append((waits, fn, (si, 16)))
        self._record(me, reads, writes)
        return me

    def finish(self, final_deps=()):
        nc = self.nc
        engmap = {'pe': 'tensor', 'act': 'scalar', 'dve': 'vector', 'pool': 'gpsimd', 'sp': 'sync'}
        sched = self
        fw = self._emit_waits('sp', set(final_deps), True)
        with nc.Block() as block:
            for e in ENGS:
                def body(engine, e=e):
                    for (waits, fn, (sk, inc)) in sched.ops[e]:
                        for (k, v) in waits:
                            engine.wait_ge(sched._semof(k), v)
                        fn(engine).then_inc(sched._semof(sk), inc)
                    if e == 'sp':
                        for (k, v) in fw:
                            engine.wait_ge(sched._semof(k), v)
                getattr(block, engmap[e])(body)
        self.ops = {e: [] for e in ENGS}


TM_PIECES = [(0, 512), (512, 1024), (1024, 1536), (2560, 3072), (3072, 3584), (3584, 3592)]
TM_OFF = [0, 512, 1024, 1536, 2048, 2560]
ZTM_W = 2568
FM_PIECES = [(1536, 2048), (2048, 2560), (3592, 4104), (4104, 4616), (4616, 5128), (5128, 5640)]


def host_consts(NT):
    c = {}
    c['ident'] = np.eye(128, dtype=np.float32)
    r = np.arange(128)
    c['tri_incl'] = (r[:, None] <= r[None, :]).astype(np.float32)
    c['tri_excl'] = (r[:, None] < r[None, :]).astype(np.float32)
    c['cmask'] = np.where(r[None, :] <= r[:, None], 0.0, NEG).astype(np.float32)
    c['cmaskT'] = np.ascontiguousarray(c['cmask'].T)
    sel = np.zeros((128, 128), np.float32); sel[127, :] = 1.0
    c['sel127'] = sel
    selh = np.zeros((4, 4, 128), np.float32)
    for h in range(4):
        selh[h, h, :] = 1.0
    c['selh'] = selh.reshape(4, 512)
    c['ones'] = np.ones((128, 128), np.float32)
    pos = (np.arange(NT * 128) - 112).astype(np.float32)
    inv = (10000.0 ** (-np.arange(32, dtype=np.float32) / 32)).astype(np.float32)
    ang = pos[:, None] * inv[None, :]
    c['cs64'] = np.concatenate([np.cos(ang), np.sin(ang)], axis=1).astype(np.float32)
    pv = np.zeros((128, 2), np.float32)
    pv[112:, 0] = 1.0
    pv[:112, 1] = NEG
    c['padv'] = pv
    return c


def build(NS, NT, GT, E, C, dbg=False):
    nc = bass.Bass("TRN2", target_bir_lowering=False)
    NTILE = NS * NT
    NTOK = NTILE * 128
    NXT = NS * (NT - 1)
    NG = NTILE // GT
    GW = GT * 128
    assert NT % GT == 0

    def din(name, shape, dt=F32):
        return nc.dram_tensor(name, list(shape), dt, kind="ExternalInput").ap()

    def dscr(name, shape, dt=F32):
        return nc.dram_tensor(name, list(shape), dt, kind="Internal").ap()

    xin = din("xin", [NTOK, D])
    w_in = din("w_in", [D, NIN])
    cwb_d = din("cwb", [128, 40])
    gate_bias = din("gate_bias", [1, 8])
    lamv = din("lamv", [1, 256])
    att_norm_g = din("att_norm_g", [1, 128]); ml_norm_g = din("ml_norm_g", [1, 512])
    w_att_out = din("w_att_out", [512, D]); w_ml_out = din("w_ml_out", [512, D]); w_o = din("w_o", [D, D])
    lnp = din("lnp", [6, D])
    w_router = din("w_router", [D, E]); b_router = din("b_router", [1, E])
    w_gu = din("w_gu", [E, D, 2048]); b_gu = din("b_gu", [E, 2048])
    w_down = din("w_down", [E, D, D]); b_down = din("b_down", [E, D])
    cst = {k: din("c_" + k, v.shape) for k, v in host_consts(NT).items()}
    eoff_d = din("eoff", [128, E]); ie1_d = din("ie1", [128, E])
    bguh = din("bguh", [E, 128, 16])

    out = nc.dram_tensor("out", [NXT * 128, D], F32, kind="ExternalOutput").ap()
    mk = (lambda n, s: nc.dram_tensor(n, list(s), F32, kind="ExternalOutput").ap()) if dbg else dscr
    H0 = mk("H0", [NTOK, D])
    ZTM = mk("ZTM", [NTOK, 1032])
    ZQKV = nc.dram_tensor("ZQKV", [NTOK, 1536], BF16, kind="Internal").ap()
    ZFM = mk("ZFM", [NTILE, 128, 8, 128])
    ZG = nc.dram_tensor("ZG", [NTILE, 128, 16, 128], BF16, kind="Internal").ap()
    YT = nc.dram_tensor("YT", [NTILE, 128, 8, 128], BF16, kind="Internal").ap()
    H1 = mk("H1", [NXT * 128, D])
    XS = nc.dram_tensor("XS", [E * C + 256, D], BF16, kind="Internal").ap()
    YS = nc.dram_tensor("YS", [E * C + 256, D], BF16, kind="Internal").ap()

    with ExitStack() as st:
        S = Sched(nc, st)

        def sb(name, shape, dt=F32):
            return st.enter_context(nc.sbuf_tensor(name, list(shape), dt))

        pb = [st.enter_context(nc.psum_tensor("pb%d" % i, [128, 512], F32)) for i in range(8)]
        prr = [0]

        def bank(lo=0, hi=8):
            i = lo + prr[0] % (hi - lo)
            prr[0] += 1
            return i

        def dma(eng, out_ap, in_ap, reads, writes, **kw):
            return S.dma(eng, lambda e: e.dma_start(out=out_ap, in_=in_ap, **kw), reads, writes)

        def mm(out_ap, lhsT, rhs, start, stop, reads, writes):
            return S.op('pe', lambda e: e.matmul(out_ap, lhsT=lhsT, rhs=rhs, start=start, stop=stop), reads, writes)

        def tr(out_ap, in_ap, reads, writes, ident_ap=None):
            idn = ident[:] if ident_ap is None else ident_ap
            return S.op('pe', lambda e: e.transpose(out_ap, in_ap, idn), list(reads) + ['ident'], writes)

        def act(out_ap, in_ap, func, reads, writes, bias=0.0, scale=1.0, accum=None):
            return S.op('act', lambda e: e.activation(out=out_ap, in_=in_ap, func=func, bias=bias, scale=scale,
                                                      **({'accum_out': accum} if accum is not None else {})),
                        reads, writes)

        def tt(eng, out_ap, a, b, op, reads, writes):
            return S.op(eng, lambda e: e.tensor_tensor(out=out_ap, in0=a, in1=b, op=op), reads, writes)

        def ts(eng, out_ap, a, s1, s2, op0, op1, reads, writes, accum=None):
            if op1 is None:
                return S.op(eng, lambda e: e.tensor_scalar(out=out_ap, in0=a, scalar1=s1, scalar2=None, op0=op0), reads, writes)
            return S.op(eng, lambda e: e.tensor_scalar(out=out_ap, in0=a, scalar1=s1, scalar2=s2, op0=op0, op1=op1,
                                                       **({'accum_out': accum} if accum is not None else {})),
                        reads, writes)

        def stt(out_ap, a, s, b, op0, op1, reads, writes):
            return S.op('dve', lambda e: e.scalar_tensor_tensor(out=out_ap, in0=a, scalar=s, in1=b, op0=op0, op1=op1),
                        reads, writes)

        def cp(eng, out_ap, in_ap, reads, writes):
            if eng == 'act':
                return S.op('act', lambda e: e.copy(out=out_ap, in_=in_ap), reads, writes)
            return S.op(eng, lambda e: e.tensor_copy(out=out_ap, in_=in_ap), reads, writes)

        ident = sb("ident", [128, 128])
        dma('sp', ident[:], cst['ident'][:, :], [], ['ident'])
        lnbh = [None]

        def load_ln(alloc, gi):
            lnbh[0] = alloc("lnb_%d" % gi, [128, 6, D]) if False else alloc("lnb_%d" % gi, [128, 2, D])
            for i in range(2):
                dma('sp', lnbh[0][:, i, :], lnp[gi + i:gi + i + 1, :].partition_broadcast(128), [], ['lnb%d' % (gi + i)])

        def layer_norm(x_ap, y_ap, gi, rd, wr, tag, tmp, stat, tmptok):
            for c2 in range(2):
                S.op('dve', lambda e, c2=c2: e.bn_stats(out=stat[:, c2 * 6:(c2 + 1) * 6], in_=x_ap[:, c2 * 512:(c2 + 1) * 512]),
                     rd, [tag + 'st%d' % c2])
            S.op('dve', lambda e: e.bn_aggr(out=stat[:, 12:14], in_=stat[:, 0:12].rearrange("p (c s) -> p c s", s=6)),
                 [tag + 'st0', tag + 'st1'], [tag + 'ag'])
            act(stat[:, 14:15], stat[:, 13:14], AF.Sqrt, [tag + 'ag'], [tag + 'sd'], bias=epsb[:, 0:1])
            S.op('dve', lambda e: e.reciprocal(out=stat[:, 15:16], in_=stat[:, 14:15]), [tag + 'sd'], [tag + 'rs'])
            stt(tmp, x_ap, stat[:, 12:13], lnbh[0][:, 0, :], ALU.subtract, ALU.mult,
                list(rd) + [tag + 'ag', 'lnb%d' % gi], [tmptok])
            stt(y_ap, tmp, stat[:, 15:16], lnbh[0][:, 1, :], ALU.mult, ALU.add, [tmptok, tag + 'rs', 'lnb%d' % (gi + 1)], wr)

        epsb = sb("epsb", [128, 1])
        S.op('dve', lambda e: e.memset(epsb[:], EPS), [], ['epsb'])

        pa = ExitStack()

        def sba(name, shape, dt=F32):
            return pa.enter_context(nc.sbuf_tensor(name, list(shape), dt))

        load_ln(sba, 0)
        h0T = sba("h0T", [128, 8, GW], F32R)
        wpc = [sba("wpc%d" % i, [128, 8, 512], F32R) for i in range(2)]
        xt = [sba("xt%d" % i, [128, D]) for i in range(4)]
        h0t = [sba("h0t%d" % i, [128, D]) for i in range(4)]
        lntmp = [sba("lntmp%d" % i, [128, D]) for i in range(4)]
        lnst = [sba("lnst%d" % i, [128, 16]) for i in range(4)]
        zst = [sba("zst%d" % i, [128, 512]) for i in range(3)]
        zsb = [sba("zsb%d" % i, [128, 512], BF16) for i in range(3)]
        rt = [sba("rt%d" % i, [128, 256]) for i in range(8)]
        csr = sba("csr", [128, NT, 64])
        csr_done = set()
        zcs = [sba("zc%d" % i, [128, 3 + GW]) for i in range(2)]
        caccs = [sba("cacc%d" % i, [128, GW]) for i in range(2)]
        fst = [sba("fst%d" % i, [128, GW]) for i in range(2)]
        fstg = [sba("fstg%d" % i, [128, GW], BF16) for i in range(2)]
        hist = sba("hist", [128, 8, 3])
        cw = sba("cw", [128, 8, 4]); cb = sba("cb", [128, 8])
        dma('sp', cw[:], cwb_d[:, 0:32].rearrange("p (c k) -> p c k", k=4), [], ['cw'])
        dma('sp', cb[:], cwb_d[:, 32:40], [], ['cb'])
        pieces = [('tm', i) for i in range(6)] + [('fm', i) for i in range(6)]
        zi = [0]
        pidx = [0]
        allp = [p for _ in range(NG) for p in pieces]

        def load_piece(gp):
            if gp >= len(allp):
                return
            kind_, pi_ = allp[gp]
            a_, b_ = (TM_PIECES if kind_ == 'tm' else FM_PIECES)[pi_]
            dma('pool', wpc[gp % 2][:, :, 0:b_ - a_], w_in[:, a_:b_].rearrange("(k p) c -> p k c", p=128), [], ['wpc%d' % (gp % 2)])

        def tm_tile(pi, g, j, wb):
            c0, c1 = TM_PIECES[pi]
            wcols = c1 - c0
            ti = g * GT + j
            bk = bank(2, 8)
            for k in range(8):
                mm(pb[bk][:, 0:wcols], h0T[:, k, j * 128:(j + 1) * 128], wpc[wb][:, k, 0:wcols], k == 0, k == 7,
                   ['h0T_%d' % j, 'wpc%d' % wb], ['pb%d' % bk])
            zb = zi[0] % 3
            zi[0] += 1
            if pi in (0, 1):
                tl = ti % NT
                if tl not in csr_done:
                    csr_done.add(tl)
                    dma('sp', csr[:, tl, :], cst['cs64'][tl * 128:(tl + 1) * 128, :], [], ['csr%d' % tl])
                zv = pb[bk][:, :].rearrange("p (g h i) -> p g h i", g=8, h=2)
                ov = zsb[zb][:, :].rearrange("p (g h i) -> p g h i", g=8, h=2)
                cv = csr[:, tl, 0:32].unsqueeze(1).to_broadcast([128, 8, 32])
                sv = csr[:, tl, 32:64].unsqueeze(1).to_broadcast([128, 8, 32])
                ro = 4 * (rtc[0] % 2)
                rtc[0] += 1
                r4 = [rt[ro + q][:, :].rearrange("p (g i) -> p g i", g=8) for q in range(4)]
                rk = ['rt%d' % (ro + q) for q in range(4)]
                rd = ['pb%d' % bk, 'csr%d' % tl]
                tt('dve', r4[0], zv[:, :, 0, :], cv, ALU.mult, rd, [rk[0]])
                tt('dve', r4[1], zv[:, :, 1, :], sv, ALU.mult, rd, [rk[1]])
                tt('dve', r4[2], zv[:, :, 1, :], cv, ALU.mult, rd, [rk[2]])
                tt('dve', r4[3], zv[:, :, 0, :], sv, ALU.mult, rd, [rk[3]])
                tt('pool', ov[:, :, 0, :], r4[0], r4[1], ALU.subtract, [rk[0], rk[1]], ['zsb%d' % zb])
                tt('pool', ov[:, :, 1, :], r4[2], r4[3], ALU.add, [rk[2], rk[3]], ['zsb%d' % zb])
            elif pi == 2:
                cp('act', zsb[zb][:, 0:wcols], pb[bk][:, 0:wcols], ['pb%d' % bk], ['zsb%d' % zb])
            else:
                cp('act', zst[zb][:, 0:wcols], pb[bk][:, 0:wcols], ['pb%d' % bk], ['zst%d' % zb])
            if pi <= 2:
                dma('sp', ZQKV[ti * 128:(ti + 1) * 128, pi * 512:pi * 512 + wcols], zsb[zb][:, 0:wcols],
                    ['zsb%d' % zb], ['ZQ%d_%d' % (pi, ti)])
            else:
                dma('sp', ZTM[ti * 128:(ti + 1) * 128, TM_OFF[pi] - 1536:TM_OFF[pi] - 1536 + wcols], zst[zb][:, 0:wcols],
                    ['zst%d' % zb], ['ZTM_%d' % ti])

        rtc = [0]
        h0T_all = ['h0T_%d' % j for j in range(GT)]
        for g in range(NG):
            if pidx[0] == 0:
                load_piece(0)
            wb0 = pidx[0] % 2
            load_piece(pidx[0] + 1)
            def a_tile(j):
                ti = g * GT + j
                q = j % 4
                T = lambda nm: '%s%d' % (nm, q)
                x_ap, stat, tmp = xt[q][:], lnst[q], lntmp[q][:]
                dma('sp', xt[q][:], xin[ti * 128:(ti + 1) * 128, :], [], [T('xt')])
                yield
                for c2 in range(2):
                    S.op('dve', lambda e, c2=c2: e.bn_stats(out=stat[:, c2 * 6:(c2 + 1) * 6], in_=x_ap[:, c2 * 512:(c2 + 1) * 512]),
                         [T('xt')], [T('lnAst%d_' % c2)])
                yield
                S.op('dve', lambda e: e.bn_aggr(out=stat[:, 12:14], in_=stat[:, 0:12].rearrange("p (c s) -> p c s", s=6)),
                     [T('lnAst0_'), T('lnAst1_')], [T('lnAag')])
                yield
                act(stat[:, 14:15], stat[:, 13:14], AF.Sqrt, [T('lnAag')], [T('lnAsd')], bias=epsb[:, 0:1])
                stt(tmp, x_ap, stat[:, 12:13], lnbh[0][:, 0, :], ALU.subtract, ALU.mult, [T('xt'), T('lnAag'), 'lnb0'], [T('lntmp')])
                yield
                S.op('dve', lambda e: e.reciprocal(out=stat[:, 15:16], in_=stat[:, 14:15]), [T('lnAsd')], [T('lnArs')])
                yield
                stt(h0t[q][:], tmp, stat[:, 15:16], lnbh[0][:, 1, :], ALU.mult, ALU.add, [T('lntmp'), T('lnArs'), 'lnb1'], [T('h0t')])
                if ti % NT == 0:
                    S.op('pool', lambda e: e.memset(h0t[q][0:112, :], 0.0), [], [T('h0t')])
                yield
                dma('sp', H0[ti * 128:(ti + 1) * 128, :], h0t[q][:], [T('h0t')], ['H0_%d' % ti])
                for kq in range(2):
                    bk = bank(0, 2)
                    for k4 in range(4):
                        k = kq * 4 + k4
                        tr(pb[bk][:, k4 * 128:(k4 + 1) * 128], h0t[q][:, k * 128:(k + 1) * 128], [T('h0t')], ['pb%d' % bk])
                    cp('act' if kq == 0 else 'dve', h0T[:, kq * 4:(kq + 1) * 4, j * 128:(j + 1) * 128],
                       pb[bk][:, :].rearrange("p (k t) -> p k t", k=4), ['pb%d' % bk], ['h0T_%d' % j])
                    yield
                tm_tile(0, g, j, wb0)
                yield

            live = []
            nxt = 0
            while nxt < GT or live:
                while nxt < GT and len(live) < 4:
                    live.append(a_tile(nxt))
                    nxt += 1
                for gnr in list(live):
                    try:
                        next(gnr)
                    except StopIteration:
                        live.remove(gnr)
            for pidx_local, (kind, pi) in enumerate(pieces):
                c0, c1 = (TM_PIECES if kind == 'tm' else FM_PIECES)[pi]
                wcols = c1 - c0
                wb = pidx[0] % 2
                pidx[0] += 1
                if pidx_local == 0:
                    continue
                load_piece(pidx[0])
                if kind == 'tm':
                    for j in range(GT):
                        tm_tile(pi, g, j, wb)
                else:
                    for cc in range(4):
                        fc = pi * 4 + cc
                        fb = fc % 2
                        zc = zcs[fb]; cacc = caccs[fb]; zck = 'zc%d' % fb; cak = 'cacc%d' % fb
                        nsub = (GW + 511) // 512
                        for sg in range(nsub):
                            t0 = sg * 512
                            n = min(512, GW - t0)
                            bk = bank(2, 8)
                            for k in range(8):
                                mm(pb[bk][:, 0:n], wpc[wb][:, k, cc * 128:(cc + 1) * 128], h0T[:, k, t0:t0 + n], k == 0, k == 7,
                                   h0T_all + ['wpc%d' % wb], ['pb%d' % bk])
                            if fc < 8:
                                cp('act', zc[:, 3 + t0:3 + t0 + n], pb[bk][:, 0:n], ['pb%d' % bk], [zck])
                            else:
                                act(fstg[fb][:, t0:t0 + n], pb[bk][:, 0:n], AF.Sigmoid, ['pb%d' % bk], ['fstg%d' % fb])
                        if fc < 8:
                            if (g * GT) % NT == 0:
                                S.op('pool', lambda e, zc=zc: e.memset(zc[:, 0:3], 0.0), [], [zck])
                            else:
                                cp('pool', zc[:, 0:3], hist[:, fc, :], ['hist%d' % fc], [zck])
                            ts('dve', cacc[:, :], zc[:, 3:3 + GW], cw[:, fc, 3:4], cb[:, fc:fc + 1], ALU.mult, ALU.add,
                               [zck, 'cw', 'cb'], [cak])
                            for tap in range(3):
                                stt(cacc[:, :], zc[:, tap:tap + GW], cw[:, fc, tap:tap + 1], cacc[:, :], ALU.mult, ALU.add,
                                    [zck, 'cw', cak], [cak])
                            cp('pool', hist[:, fc, :], zc[:, GW:GW + 3], [zck], ['hist%d' % fc])
                            act(fst[fb][:, :], cacc[:, :], AF.Silu, [cak], ['fst%d' % fb],
                                )
                            if fc >= 4:
                                ts('dve', fst[fb][:, :], fst[fb][:, :], 128.0 ** -0.5, None, ALU.mult, None, ['fst%d' % fb], ['fst%d' % fb])
                        if fc < 8:
                            dma('sp', ZFM[g * GT:(g + 1) * GT, :, fc, :].rearrange("j p t -> p j t"),
                                fst[fb][:, :].rearrange("p (j t) -> p j t", j=GT), ['fst%d' % fb],
                                ['ZFM_%d' % t_ for t_ in range(g * GT, (g + 1) * GT)])
                        else:
                            dma('sp', ZG[g * GT:(g + 1) * GT, :, fc - 8, :].rearrange("j p t -> p j t"),
                                fstg[fb][:, :].rearrange("p (j t) -> p j t", j=GT), ['fstg%d' % fb],
                                ['ZG_%d' % t_ for t_ in range(g * GT, (g + 1) * GT)])
        S.finish()
        S.barrier()
        pa.close()

        gates_all = sb("gates_all", [128, NXT, 4]); slots_all = sb("slots_all", [128, NXT, 4], I32)
        pw = ExitStack()
        wa = pw.enter_context(nc.sbuf_tensor("wa", [128, 4, D], BF16)); wm_ = pw.enter_context(nc.sbuf_tensor("wm", [128, 4, D], BF16))
        wo = pw.enter_context(nc.sbuf_tensor("wo", [128, 8, D], BF16))
        dma('pool', wa[:], w_att_out.rearrange("(k p) c -> p k c", p=128), [], ['wa'])
        dma('pool', wm_[:], w_ml_out.rearrange("(k p) c -> p k c", p=128), [], ['wm'])
        for k2 in range(2):
            dma('pool', wo[:, k2 * 4:(k2 + 1) * 4, :], w_o[k2 * 512:(k2 + 1) * 512, :].rearrange("(k p) c -> p k c", p=128), [], ['wo'])
        p1 = ExitStack()

        def sb1(name, shape, dt=F32):
            return p1.enter_context(nc.sbuf_tensor(name, list(shape), dt))

        KMAX = 16 + (NT - 1) * 128
        KT = sb1("KT", [128, 4, KMAX], BF16)
        Vc = sb1("Vc", [128, NT, 4, 129], BF16)
        qkin = [sb1("qkin%d" % i, [128, 1024], BF16) for i in range(2)]
        identb = sb1("identb", [128, 128], BF16)
        cp('dve', identb[:], ident[:], ['ident'], ['identb'])
        qT = sb1("qT", [128, 4, 128], BF16)
        PTb = [sb1("PTb%d" % i, [128, 512], BF16) for i in range(6)]
        cmaskT = sb1("cmaskT", [128, 128]); onesb1 = sb1("onesb1", [8, 128]); sqt = sb1("sqt", [128, 512])
        nrm = sb1("nrm", [128, 32]); negc = sb1("negc", [128, 8]); kmx = sb1("kmx", [8, 8]); dg8 = sb1("dg8", [8, 8])
        osb = sb1("osb", [128, 512]); otmp = sb1("otmp", [128, 512])
        mxc = sb1("mxc", [128, 8, 16]); smc = sb1("smc", [128, 8, 16])
        att_s = sb1("att_s", [128, 64])
        cmask = sb1("cmask", [128, 128])
        dma('sp', cmask[:], cst['cmask'][:, :], [], ['cmask'])
        dma('sp', cmaskT[:], cst['cmaskT'][:, :], [], ['cmaskT'])
        dma('sp', onesb1[:], cst['ones'][0:8, :], [], ['onesb1'])
        S.op('pool', lambda e: e.memset(Vc[:, :, :, 128:129], 1.0), [], ['Vones'])
        lamb = sb1("lamb", [128, 256]); lams = sb1("lams", [128, 8])
        dma('sp', lamb[:], lamv.partition_broadcast(128), [], ['lamb'])
        tt('dve', lamb[:, 0:64], lamb[:, 0:64], lamb[:, 64:128], ALU.mult, ['lamb'], ['lamb'])
        tt('dve', lamb[:, 128:192], lamb[:, 128:192], lamb[:, 192:256], ALU.mult, ['lamb'], ['lamb'])
        S.op('dve', lambda e: e.reduce_sum(out=lams[:, 0:1], in_=lamb[:, 0:64], axis=AX.X), ['lamb'], ['lams'])
        S.op('dve', lambda e: e.reduce_sum(out=lams[:, 1:2], in_=lamb[:, 128:192], axis=AX.X), ['lamb'], ['lams'])
        act(lams[:, 2:4], lams[:, 0:2], AF.Exp, ['lams'], ['lams'])
        tt('dve', lams[:, 4:5], lams[:, 3:4], lams[:, 2:3], ALU.subtract, ['lams'], ['lams'])
        ts('dve', lams[:, 5:6], lams[:, 4:5], -LAMBDA_INIT, None, ALU.add, None, ['lams'], ['lams'])
        gatt = sb1("gatt", [128, 128])
        dma('sp', gatt[:], att_norm_g.partition_broadcast(128), [], ['gatt'])
        ts('dve', gatt[:], gatt[:], 1.0 - LAMBDA_INIT, None, ALU.mult, None, ['gatt'], ['gatt'])
        yatt = sb1("yatt", [128, 512])
        ytile = [sb1("ytile%d" % i, [128, 8, 128], BF16) for i in range(2)]

        tri_incl = sb1("tri_incl", [128, 128]); sel127 = sb1("sel127", [128, 128]); selh = sb1("selh", [4, 512])
        padv = sb1("padv", [128, 2]); gbias = sb1("gbias", [128, 8]); mlg = sb1("mlg", [128, 512])
        dma('sp', tri_incl[:], cst['tri_incl'][:, :], [], ['tri_incl'])
        dma('sp', sel127[:], cst['sel127'][:, :], [], ['sel127'])
        dma('sp', selh[:], cst['selh'][:, :], [], ['selh'])
        dma('sp', padv[:], cst['padv'][:, :], [], ['padv'])
        dma('sp', gbias[:], gate_bias.partition_broadcast(128), [], ['gbias'])
        dma('sp', mlg[:], ml_norm_g.partition_broadcast(128), [], ['mlg'])
        CT = sb1("CT", [128, 4, 129]); mbc = sb1("mbc", [128, 4])
        fmq = [sb1("fmq%d" % i, [128, 8, 128]) for i in range(2)]
        mvo = [sb1("mvo%d" % i, [128, 1032]) for i in range(2)]
        ms = sb1("ms", [128, 96])
        MB = sb1("MB", [128, 8]); bcs = sb1("bcs", [128, 8])
        gT = sb1("gT", [4, 128])
        Gm = sb1("Gm", [128, 4, 128]); Dm = Gm; Sm = Gm
        STm = sb1("STm", [128, 4, 128]); svs = sb1("svs", [128, 512]); numt = sb1("numt", [128, 4, 128])
        sgm = svs; yml = yatt; vwx = sb1("vwx", [128, 4, 129])
        ktm = STm; junk = sb1("junk", [128, 128])

        def mlstm_tile(sq, t, ti, b2):
            r0 = ti * 128
            if t == 0:
                S.op('dve', lambda e: e.memset(CT[:], 0.0), [], ['CT'])
                S.op('dve', lambda e: e.memset(mbc[:], 0.0), [], ['mbc'])
            mqT = lambda h: fmq[b2][:, h, :]
            mkT = lambda h: fmq[b2][:, 4 + h, :]
            mv = lambda h: mvo[b2][:, h * 128:(h + 1) * 128]
            X = lambda a, b=None: ms[:, a:(a + 4 if b is None else b)]
            fq, mvt = 'fmq%d' % b2, 'mvo%d' % b2
            tt('dve', X(0, 8), mvo[b2][:, 1024:1032], gbias[:], ALU.add, [mvt, 'gbias'], ['m_gt'])
            stt(X(8), X(4), -1.0, X(4), ALU.mult, ALU.max, ['m_gt'], ['m_ax'])
            act(X(8), X(8), AF.Exp, ['m_ax'], ['m_ax'], scale=-1.0)
            act(X(8), X(8), AF.Ln, ['m_ax'], ['m_ax'], bias=1.0)
            ts('dve', X(12), X(4), 0.0, None, ALU.min, None, ['m_gt'], ['m_lf'])
            tt('dve', X(12), X(12), X(8), ALU.subtract, ['m_lf', 'm_ax'], ['m_lf'])
            if t == 0:
                ts('dve', X(0), X(0), padv[:, 0:1], padv[:, 1:2], ALU.mult, ALU.add, ['m_gt', 'padv'], ['m_gt'])
                ts('dve', X(12), X(12), padv[:, 0:1], None, ALU.mult, None, ['m_lf', 'padv'], ['m_lf'])
            yield
            bk = bank(0, 2)
            mm(pb[bk][:, 0:4], tri_incl[:], X(12), True, True, ['tri_incl', 'm_lf'], ['pb%d' % bk])
            cp('dve', MB[:, 4:8], pb[bk][:, 0:4], ['pb%d' % bk], ['MBb'])
            tt('dve', X(16), X(0), MB[:, 4:8], ALU.subtract, ['m_gt', 'MBb'], ['m_g'])
            yield
            bk = bank(0, 2)
            tr(pb[bk][0:4, 0:128], X(16), ['m_g'], ['pb%d' % bk])
            cp('act', gT[:, :], pb[bk][0:4, 0:128], ['pb%d' % bk], ['gT'])
            yield
            bg = sbank()
            for h in range(4):
                mm(pb[bg][:, h * 128:(h + 1) * 128], selh[0:4, h * 128:(h + 1) * 128], gT[0:4, :], True, True, ['selh', 'gT'], ['pb%d' % bg])
            for h in range(4):
                tt('dve', Gm[:, h, :], pb[bg][:, h * 128:(h + 1) * 128], cmask[:], ALU.add, ['pb%d' % bg, 'cmask'], ['Gm'])
            yield
            S.op('dve', lambda e: e.reduce_max(out=X(20), in_=Gm[:, :, :], axis=AX.X), ['Gm'], ['m_cm'])
            tt('dve', MB[:, 0:4], X(20), mbc[:], ALU.max, ['m_cm', 'mbc'], ['MBm'])
            if t > 0:
                ts('dve', X(24), MB[:, 0:4], -1.0, None, ALU.mult, None, ['MBm'], ['m_negM'])
                for h in range(4):
                    act(Dm[:, h, :], Gm[:, h, :], AF.Exp, ['Gm', 'm_negM'], ['Gm'], bias=ms[:, 24 + h:25 + h])
                yield
                bq = sbank()
                for h in range(4):
                    mm(pb[bq][:, h * 128:(h + 1) * 128], mqT(h), mkT(h), True, True, [fq], ['pb%d' % bq])
                tt('dve', Sm[:, :, :], pb[bq][:, :].rearrange("p (h r) -> p h r", h=4), Dm[:, :, :], ALU.mult, ['pb%d' % bq, 'Gm'], ['Gm'])
                S.op('dve', lambda e: e.reduce_sum(out=X(28), in_=Sm[:, :, :], axis=AX.X), ['Gm'], ['m_rs'])
                yield
                bt = sbank()
                for h in range(4):
                    tr(pb[bt][:, h * 128:(h + 1) * 128], Sm[:, h, :], ['Gm'], ['pb%d' % bt])
                cp('act', STm[:, :, :], pb[bt][:, :].rearrange("p (h r) -> p h r", h=4), ['pb%d' % bt], ['STm'])
                yield
                bo = sbank()
                for h in range(4):
                    mm(pb[bo][:, h * 128:(h + 1) * 128], STm[:, h, :], mv(h), True, True, ['STm', mvt], ['pb%d' % bo])
                cp('act', svs[:, :], pb[bo][:, :], ['pb%d' % bo], ['svs'])
                bu = [sbank(), sbank()]
                for h in range(4):
                    mm(pb[bu[h // 2]][:, (h % 2) * 129:(h % 2) * 129 + 129], mqT(h), CT[:, h, :], True, True, [fq, 'CT'], ['pb%d' % bu[h // 2]])
                tt('dve', X(32), mbc[:], MB[:, 0:4], ALU.subtract, ['mbc', 'MBm'], ['m_wi'])
                act(X(32), X(32), AF.Exp, ['m_wi'], ['m_wi'])
                for h in range(4):
                    stt(numt[:, h, :], pb[bu[h // 2]][:, (h % 2) * 129:(h % 2) * 129 + 128], ms[:, 32 + h:33 + h], svs[:, h * 128:(h + 1) * 128],
                        ALU.mult, ALU.add, ['pb%d' % bu[h // 2], 'm_wi', 'svs'], ['numt'])
                for hh in range(2):
                    cp('dve', ms[:, 36 + 2 * hh:38 + 2 * hh], pb[bu[hh]][:, 128:258:129], ['pb%d' % bu[hh]], ['m_uc%d' % hh])
                tt('dve', X(36), X(36), X(32), ALU.mult, ['m_uc0', 'm_uc1', 'm_wi'], ['m_den'])
                tt('dve', X(36), X(36), X(28), ALU.add, ['m_den', 'm_rs'], ['m_den'])
                stt(X(36), X(36), -1.0, X(36), ALU.mult, ALU.max, ['m_den'], ['m_den'])
                tt('dve', X(40), MB[:, 0:4], MB[:, 4:8], ALU.add, ['MBm', 'MBb'], ['m_em'])
                act(X(40), X(40), AF.Exp, ['m_em'], ['m_em'], scale=-1.0)
                tt('dve', X(36), X(36), X(40), ALU.max, ['m_den', 'm_em'], ['m_den'])
                S.op('dve', lambda e: e.reciprocal(out=X(44), in_=X(36)), ['m_den'], ['m_rden'])
                yield
                tt('dve', Gm[:, :, :], numt[:, :, :], numt[:, :, :], ALU.mult, ['numt', 'Gm'], ['Gm'])
                S.op('dve', lambda e: e.reduce_sum(out=X(48), in_=Gm[:, :, :], axis=AX.X), ['Gm'], ['m_ssq'])
                tt('dve', X(52), X(44), X(44), ALU.mult, ['m_rden'], ['m_q2'])
                tt('dve', X(52), X(52), X(48), ALU.mult, ['m_q2', 'm_ssq'], ['m_q2'])
                ts('dve', X(52), X(52), 1.0 / 128, EPS, ALU.mult, ALU.add, ['m_q2'], ['m_q2'])
                act(X(52), X(52), AF.Ln, ['m_q2'], ['m_q2'])
                act(X(52), X(52), AF.Exp, ['m_q2'], ['m_q2'], scale=-0.5)
                tt('dve', X(56), X(52), X(44), ALU.mult, ['m_q2', 'm_rden'], ['m_scl'])
                yield
                act(sgm[:, :], mvo[b2][:, 512:1024], AF.Exp, [mvt], ['svs'], scale=-1.0)
                ts('dve', sgm[:, :], sgm[:, :], 1.0, None, ALU.add, None, ['svs'], ['svs'])
                S.op('dve', lambda e: e.reciprocal(out=sgm[:, :], in_=sgm[:, :]), ['svs'], ['svs'])
                tt('pool', sgm[:, :], sgm[:, :], mlg[:], ALU.mult, ['svs', 'mlg'], ['svs'])
                for h in range(4):
                    stt(yml[:, h * 128:(h + 1) * 128], numt[:, h, :], ms[:, 56 + h:57 + h], sgm[:, h * 128:(h + 1) * 128],
                        ALU.mult, ALU.mult, ['numt', 'm_scl', 'svs'], ['yatt'])
                yield
                bk = bank(0, 2)
                for h in range(4):
                    tr(pb[bk][:, h * 128:(h + 1) * 128], yml[:, h * 128:(h + 1) * 128], ['yatt'], ['pb%d' % bk])
                cp('act', ytile[b2][:, 4:8, :], pb[bk][:, :].rearrange("p (h t) -> p h t", h=4), ['pb%d' % bk], ['ytile%d' % b2])
                dma('sp', YT[ti, :, 4:8, :], ytile[b2][:, 4:8, :], ['ytile%d' % b2], ['YTm_%d' % ti])
            if t == NT - 1:
                return
            yield
            yield
            bk = bank(0, 2)
            mm(pb[bk][:, 0:8], sel127[:], MB[:, :], True, True, ['sel127', 'MBm', 'MBb'], ['pb%d' % bk])
            cp('dve', bcs[:, :], pb[bk][:, 0:8], ['pb%d' % bk], ['bcs'])
            tt('dve', X(60), X(16), bcs[:, 0:4], ALU.subtract, ['m_g', 'bcs'], ['m_wr'])
            act(X(60), X(60), AF.Exp, ['m_wr'], ['m_wr'])
            tt('dve', X(64), mbc[:], bcs[:, 0:4], ALU.subtract, ['mbc', 'bcs'], ['m_wo'])
            act(X(64), X(64), AF.Exp, ['m_wo'], ['m_wo'])
            tt('dve', mbc[:], bcs[:, 4:8], bcs[:, 0:4], ALU.add, ['bcs'], ['mbc'])
            yield
            for h in range(4):
                ts('pool', vwx[:, h, 0:128], mv(h), ms[:, 60 + h:61 + h], 1.0, ALU.mult, ALU.mult, [mvt, 'm_wr'], ['vwx'])
            cp('pool', vwx[:, :, 128:129], X(60).rearrange("p (h o) -> p h o", o=1), ['m_wr'], ['vwx'])
            bt = sbank()
            for h in range(4):
                tr(pb[bt][:, h * 128:(h + 1) * 128], mkT(h), [fq], ['pb%d' % bt])
            cp('act', ktm[:, :, :], pb[bt][:, :].rearrange("p (h r) -> p h r", h=4), ['pb%d' % bt], ['STm'])
            yield
            bc2 = [sbank(), sbank()]
            for h in range(4):
                mm(pb[bc2[h // 2]][:, (h % 2) * 129:(h % 2) * 129 + 129], ktm[:, h, :], vwx[:, h, :], True, True, ['STm', 'vwx'], ['pb%d' % bc2[h // 2]])
            for h in range(4):
                stt(CT[:, h, :], CT[:, h, :], ms[:, 64 + h:65 + h], pb[bc2[h // 2]][:, (h % 2) * 129:(h % 2) * 129 + 129],
                    ALU.mult, ALU.add, ['CT', 'm_wo', 'pb%d' % bc2[h // 2]], ['CT'])

        def kchunks(nk):
            bounds = [0]
            nxt = 400
            while nxt < nk:
                bounds.append(nxt)
                nxt += 512
            bounds.append(nk)
            return [(bounds[i], bounds[i + 1]) for i in range(len(bounds) - 1)]

        sbk = [0]
        stc = [0]; ptc = [0]
        zt = sb1("zt", [128, 8192], BF16)
        S.op('pool', lambda e: e.memset(zt[:], 0.0), [], ['zt'])
        XSv = XS[0:E * C, :].rearrange("(p r) d -> p (r d)", p=128)
        per_p = (E * C // 128) * D
        zf_chunks = [(c0_, min(8192, per_p - c0_)) for c0_ in range(0, per_p, 8192)]
        zf_per_tile = -(-len(zf_chunks) // max(1, NTILE - 1))
        zf_i = [0]

        def zero_fill_some():
            for _ in range(zf_per_tile):
                if zf_i[0] < len(zf_chunks):
                    c0_, n_ = zf_chunks[zf_i[0]]
                    zf_i[0] += 1
                    dma('sp', XSv[:, c0_:c0_ + n_], zt[:, 0:n_], ['zt'], ['XS0_%d' % c0_])

        def head_done(h):
            rg = (h % 2) * 129
            hs = slice(h * 128, (h + 1) * 128)
            S.op('dve', lambda e: e.reciprocal(out=att_s[:, 2 * h:2 * h + 1], in_=pb[6][:, rg + 128:rg + 129]), ['pb6'], ['att_ra%d' % h])
            S.op('dve', lambda e: e.reciprocal(out=att_s[:, 2 * h + 1:2 * h + 2], in_=pb[7][:, rg + 128:rg + 129]), ['pb7'], ['att_rb%d' % h])
            tt('dve', att_s[:, 8 + h:9 + h], att_s[:, 2 * h + 1:2 * h + 2], lams[:, 5:6], ALU.mult, ['att_rb%d' % h, 'lams'], ['att_c%d' % h])
            ts('dve', otmp[:, hs], pb[7][:, rg:rg + 128], att_s[:, 8 + h:9 + h], None, ALU.mult, None, ['pb7', 'att_c%d' % h], ['otmp'])
            stt(osb[:, hs], pb[6][:, rg:rg + 128], att_s[:, 2 * h:2 * h + 1], otmp[:, hs], ALU.mult, ALU.add, ['pb6', 'att_ra%d' % h, 'otmp'], ['osb'])

        def sbank():
            sbk[0] += 1
            return 2 + sbk[0] % 2

        def issue_loads(tj):
            tq = tj % NT
            bq = tj % 2
            rq = tj * 128
            zq = ['ZQ%d_%d' % (p_, tj) for p_ in range(3)]
            dma('sp', qkin[bq][:], ZQKV[rq:rq + 128, 0:1024], zq, ['qkin%d' % bq])
            dma('sp', fmq[bq][:], ZFM[tj, :, 0:8, :], ['ZFM_%d' % tj], ['fmq%d' % bq])
            dma('sp', mvo[bq][:], ZTM[rq:rq + 128, 0:1032], ['ZTM_%d' % tj], ['mvo%d' % bq])
            if tq == 0:
                dma('sp', Vc[0:16, 0, :, 0:128], ZQKV[rq + 112:rq + 128, 1024:1536].rearrange("p (h e) -> p h e", h=4),
                    zq + ['Vones'], ['Vc%d' % tq])
            else:
                dma('sp', Vc[:, tq, :, 0:128], ZQKV[rq:rq + 128, 1024:1536].rearrange("p (h e) -> p h e", h=4),
                    zq + ['Vones'], ['Vc%d' % tq])

        for sq in range(NS):
            for t in range(NT):
                ti = sq * NT + t
                b2 = ti % 2
                r0 = ti * 128
                if ti == 0:
                    issue_loads(0)
                if ti + 1 < NTILE:
                    issue_loads(ti + 1)
                mgen = mlstm_tile(sq, t, ti, b2)
                next(mgen, None)
                if t == 0:
                    kc0, kn = 0, 16
                else:
                    kc0, kn = 16 + (t - 1) * 128, 128
                bk = bank(0, 2)
                pvk = pb[bk][:, :].bitcast(BF16)[:, 0:512]
                for h in range(4):
                    S.op('pe', lambda e, h=h, pvk=pvk, b2=b2: e.transpose(pvk[:, h * 128:(h + 1) * 128], qkin[b2][:, 512 + h * 128:512 + (h + 1) * 128], identb[:]),
                         ['qkin%d' % b2, 'identb'], ['pb%d' % bk])
                cp('act', KT[:, :, kc0:kc0 + kn], pvk.rearrange("p (h t) -> p h t", h=4)[:, :, 128 - kn:128],
                   ['pb%d' % bk], ['KT'])
                tt('dve', sqt[:], qkin[b2][:, 512:1024], qkin[b2][:, 512:1024], ALU.mult, ['qkin%d' % b2], ['sqt'])
                S.op('dve', lambda e: e.reduce_sum(out=nrm[:, 0:8], in_=sqt[:, :].rearrange("p (g d) -> p g d", g=8), axis=AX.X), ['sqt'], ['nrm_k'])
                bk = bank(0, 2)
                tr(pb[bk][0:8, 0:128], nrm[:, 0:8], ['nrm_k'], ['pb%d' % bk])
                if t == 0:
                    S.op('dve', lambda e, bk=bk: e.reduce_max(out=kmx[:, 0:1], in_=pb[bk][0:8, 0:128], axis=AX.X), ['pb%d' % bk], ['kmx'])
                else:
                    S.op('dve', lambda e, bk=bk: e.reduce_max(out=kmx[:, 1:2], in_=pb[bk][0:8, 0:128], axis=AX.X), ['pb%d' % bk], ['kmx1'])
                    tt('dve', kmx[:, 0:1], kmx[:, 0:1], kmx[:, 1:2], ALU.max, ['kmx', 'kmx1'], ['kmx'])
                if t == 0:
                    for _ in mgen:
                        pass
                    continue
                bk = bank(0, 2)
                pvq = pb[bk][:, :].bitcast(BF16)[:, 0:512]
                for h in range(4):
                    S.op('pe', lambda e, h=h, pvq=pvq, b2=b2: e.transpose(pvq[:, h * 128:(h + 1) * 128], qkin[b2][:, h * 128:(h + 1) * 128], identb[:]),
                         ['qkin%d' % b2, 'identb'], ['pb%d' % bk])
                cp('act', qT[:, :, :], pvq.rearrange("p (h t) -> p h t", h=4), ['pb%d' % bk], ['qT'])
                tt('dve', sqt[:], qkin[b2][:, 0:512], qkin[b2][:, 0:512], ALU.mult, ['qkin%d' % b2], ['sqt'])
                S.op('dve', lambda e: e.reduce_sum(out=nrm[:, 8:16], in_=sqt[:, :].rearrange("p (g d) -> p g d", g=8), axis=AX.X), ['sqt'], ['nrm_q'])
                bk = bank(0, 2)
                tr(pb[bk][0:8, 0:128], nrm[:, 8:16], ['nrm_q'], ['pb%d' % bk])
                S.op('dve', lambda e, bk=bk: e.reduce_max(out=kmx[:, 2:3], in_=pb[bk][0:8, 0:128], axis=AX.X), ['pb%d' % bk], ['qmx'])
                tt('dve', kmx[:, 3:4], kmx[:, 2:3], kmx[:, 0:1], ALU.mult, ['qmx', 'kmx'], ['cprod'])
                act(kmx[:, 3:4], kmx[:, 3:4], AF.Ln, ['cprod'], ['cprod'])
                act(kmx[:, 3:4], kmx[:, 3:4], AF.Exp, ['cprod'], ['cprod'], scale=0.5)
                ts('dve', kmx[:, 4:5], kmx[:, 3:4], -0.125, None, ALU.mult, None, ['cprod'], ['cneg'])
                ts('dve', dg8[:, :], ident[0:8, 0:8], kmx[:, 4:5], None, ALU.mult, None, ['cneg', 'ident'], ['dg8'])
                bk = bank(0, 2)
                mm(pb[bk][:, 0:8], onesb1[0:8, :], dg8[0:8, 0:8], True, True, ['onesb1', 'dg8'], ['pb%d' % bk])
                cp('dve', negc[:, :], pb[bk][:, 0:8], ['pb%d' % bk], ['negc'])
                blocks = [(0, 16, 0)] + [(16 + (j - 1) * 128, 128, j) for j in range(1, t + 1)]
                groups = [blocks[g0:g0 + 4] for g0 in range(0, len(blocks), 4)]

                def st_exp(h, grp):
                    pr = stc[0] % 2
                    stc[0] += 1
                    res = []
                    bks = (2 + 2 * pr, 3 + 2 * pr)
                    for i, (c0, n, j) in enumerate(grp):
                        for m in range(2):
                            ps_ = slice(m * 64, (m + 1) * 64)
                            mm(pb[bks[m]][0:n, i * 128:(i + 1) * 128], KT[ps_, h, c0:c0 + n], qT[ps_, h, :], True, True, ['KT', 'qT'], ['pb%d' % bks[m]])
                    for m in range(2):
                        hm = h * 2 + m
                        bk = bks[m]
                        slot = ptc[0] % 6
                        ptc[0] += 1
                        ptok = 'PTb%d' % slot
                        if grp[-1][2] == t:
                            i = len(grp) - 1
                            tt('dve', pb[bk][:, i * 128:(i + 1) * 128], pb[bk][:, i * 128:(i + 1) * 128], cmaskT[:], ALU.add,
                               ['pb%d' % bk, 'cmaskT'], ['pb%d' % bk])
                        lo = 0
                        if grp[0][1] == 16:
                            act(PTb[slot][0:16, 0:128], pb[bk][0:16, 0:128], AF.Exp, ['pb%d' % bk, 'negc'], [ptok], bias=negc[0:16, hm:hm + 1], scale=0.125)
                            lo = 128
                        hi = 128 * len(grp)
                        if hi > lo:
                            act(PTb[slot][:, lo:hi], pb[bk][:, lo:hi], AF.Exp, ['pb%d' % bk, 'negc'], [ptok], bias=negc[:, hm:hm + 1], scale=0.125)
                        res.append((slot, ptok))
                    return res

                def pv(h, grp, res):
                    rg = (h % 2) * 129
                    for m in range(2):
                        slot, ptok = res[m]
                        for i, (c0, n, j) in enumerate(grp):
                            mm(pb[6 + m][:, rg:rg + 129], PTb[slot][0:n, i * 128:(i + 1) * 128], Vc[0:n, j, h, :],
                               j == 0, j == t, [ptok, 'Vc%d' % j], ['pb%d' % (6 + m)])

                work = [(h, grp) for h in range(4) for grp in groups]
                prev = None
                for wi_, (h, grp) in enumerate(work):
                    res = st_exp(h, grp)
                    if prev is not None:
                        pv(*prev)
                        if prev[0] != h:
                            head_done(prev[0])
                    prev = (h, grp, res)
                    if wi_ == 1:
                        zero_fill_some()
                    if wi_ % 2 == 1:
                        next(mgen, None)
                pv(*prev)
                head_done(3)
                for _ in mgen:
                    pass
                tt('dve', otmp[:, :], osb[:, :], osb[:, :], ALU.mult, ['osb', 'otmp'], ['otmp'])
                S.op('dve', lambda e: e.reduce_sum(out=att_s[:, 40:44], in_=otmp[:, :].rearrange("p (h e) -> p h e", h=4), axis=AX.X), ['otmp'], ['att_ss'])
                ts('dve', att_s[:, 40:44], att_s[:, 40:44], 1.0 / 128, EPS, ALU.mult, ALU.add, ['att_ss'], ['att_ss'])
                act(att_s[:, 40:44], att_s[:, 40:44], AF.Ln, ['att_ss'], ['att_ss'])
                act(att_s[:, 40:44], att_s[:, 40:44], AF.Exp, ['att_ss'], ['att_ss'], scale=-0.5)
                for h in range(4):
                    stt(yatt[:, h * 128:(h + 1) * 128], osb[:, h * 128:(h + 1) * 128], att_s[:, 40 + h:41 + h], gatt[:],
                        ALU.mult, ALU.mult, ['osb', 'att_ss', 'gatt'], ['yatt'])
                bk = bank(0, 2)
                for h in range(4):
                    tr(pb[bk][:, h * 128:(h + 1) * 128], yatt[:, h * 128:(h + 1) * 128], ['yatt'], ['pb%d' % bk])
                cp('act', ytile[b2][:, 0:4, :], pb[bk][:, :].rearrange("p (h t) -> p h t", h=4), ['pb%d' % bk], ['ytile%d' % b2])
                dma('sp', YT[ti, :, 0:4, :], ytile[b2][:, 0:4, :], ['ytile%d' % b2], ['YTa_%d' % ti])
        S.finish()
        S.barrier()
        p1.close()


        p2 = ExitStack()

        def sb2(name, shape, dt=F32):
            return p2.enter_context(nc.sbuf_tensor(name, list(shape), dt))

        QG = min(4, NT - 1)
        QW = QG * 128
        assert (NT - 1) % QG == 0
        load_ln(sb2, 2)
        wrt = sb2("wrt", [128, 8, E]); brt = sb2("brt", [1, E]); ones = sb2("ones", [128, 128])
        dma('sp', wrt[:], w_router.rearrange("(k p) e -> p k e", p=128), [], ['wrt'])
        dma('sp', brt[:], b_router[:, :], [], ['brt'])
        dma('sp', ones[:], cst['ones'][:, :], [], ['ones'])
        tri_excl = sb2("tri_excl", [128, 128]); eoff = sb2("eoff_s", [128, E]); ie1 = sb2("ie1_s", [128, E])
        dma('sp', tri_excl[:], cst['tri_excl'][:, :], [], ['tri_excl'])
        dma('sp', eoff[:], eoff_d[:, :], [], ['eoff'])
        dma('sp', ie1[:], ie1_d[:, :], [], ['ie1'])
        basecnt = sb2("basecnt", [128, E])
        S.op('dve', lambda e: e.memset(basecnt[:], 0.0), [], ['basecnt'])
        yin = sb2("yin0", [128, 8, QW], BF16)
        gin = sb2("gin0", [128, 16, QW], BF16)
        mrgT = sb2("mrgT", [128, 8, QW], BF16)
        t12 = [sb2("t12_%d" % i, [128, QW]) for i in range(4)]
        NJ = QG
        h0in = [sb2("h0in%d" % i, [128, D]) for i in range(NJ)]
        r1 = [sb2("r1_%d" % i, [128, D]) for i in range(NJ)]
        lntmp2 = [sb2("lntmp2_%d" % i, [128, D]) for i in range(NJ)]
        lnst2 = [sb2("lnst2_%d" % i, [128, 16]) for i in range(NJ)]
        h1 = [sb2("h1_%d" % i, [128, D]) for i in range(NJ)]
        h1T = [sb2("h1T%d" % i, [128, 8, 128]) for i in range(NJ)]
        h1b = [sb2("h1b%d" % i, [128, D], BF16) for i in range(NJ)]
        rsl = [sb2("rs_%d" % i, [128, 64]) for i in range(NJ)]
        lgl = [sb2("lg%d" % i, [128, E]) for i in range(NJ)]; mskl = [sb2("msk%d" % i, [128, E]) for i in range(NJ)]
        exgl = [sb2("exg%d" % i, [128, E]) for i in range(NJ)]; posl = [sb2("posf%d" % i, [128, E]) for i in range(NJ)]
        ohel = [sb2("ohe%d" % i, [128, E]) for i in range(NJ)]
        xs_tokens = []
        xs0_tokens = [k for k in S.last_w if k.startswith('XS0_')]

        def b2_tile(sq, gq, j):
            q = j
            ti = sq * NT + 1 + gq * QG + j
            xi = sq * (NT - 1) + gq * QG + j
            T = lambda nm: '%s_%d' % (nm, q)
            rs_, lg, msk, exg, posf, ohe = rsl[q], lgl[q], mskl[q], exgl[q], posl[q], ohel[q]
            dma('sp', h0in[q][:], H0[ti * 128:(ti + 1) * 128, :], ['H0_%d' % ti], [T('h0in')])
            yield
            for half in range(2):
                bk = bank(2, 8)
                for kc in range(8):
                    mm(pb[bk][:, :], mrgT[:, kc, j * 128:(j + 1) * 128], wo[:, kc, half * 512:(half + 1) * 512], kc == 0, kc == 7,
                       ['mrgT', 'wo'], ['pb%d' % bk])
                stt(r1[q][:, half * 512:(half + 1) * 512], h0in[q][:, half * 512:(half + 1) * 512], ALPHA, pb[bk][:, :],
                    ALU.mult, ALU.add, [T('h0in'), 'pb%d' % bk], [T('r1')])
            yield
            x_ap, stat, tmp = r1[q][:], lnst2[q], lntmp2[q][:]
            for c2 in range(2):
                S.op('dve', lambda e, c2=c2: e.bn_stats(out=stat[:, c2 * 6:(c2 + 1) * 6], in_=x_ap[:, c2 * 512:(c2 + 1) * 512]),
                     [T('r1')], [T('st%d' % c2)])
            yield
            S.op('dve', lambda e: e.bn_aggr(out=stat[:, 12:14], in_=stat[:, 0:12].rearrange("p (c s) -> p c s", s=6)),
                 [T('st0'), T('st1')], [T('ag')])
            yield
            act(stat[:, 14:15], stat[:, 13:14], AF.Sqrt, [T('ag')], [T('sd')], bias=epsb[:, 0:1])
            yield
            S.op('dve', lambda e: e.reciprocal(out=stat[:, 15:16], in_=stat[:, 14:15]), [T('sd')], [T('rsd')])
            stt(tmp, x_ap, stat[:, 12:13], lnbh[0][:, 0, :], ALU.subtract, ALU.mult, [T('r1'), T('ag'), 'lnb2'], [T('lntmp')])
            yield
            stt(h1[q][:], tmp, stat[:, 15:16], lnbh[0][:, 1, :], ALU.mult, ALU.add, [T('lntmp'), T('rsd'), 'lnb3'], [T('h1')])
            dma('sp', H1[xi * 128:(xi + 1) * 128, :], h1[q][:], [T('h1')], ['H1_%d' % xi])
            cp('act', h1b[q][:], h1[q][:], [T('h1')], [T('h1b')])
            yield
            for kq in range(2):
                bk = bank(0, 2)
                for k4 in range(4):
                    k = kq * 4 + k4
                    tr(pb[bk][:, k4 * 128:(k4 + 1) * 128], h1[q][:, k * 128:(k + 1) * 128], [T('h1')], ['pb%d' % bk])
                cp('act' if kq == 0 else 'dve', h1T[q][:, kq * 4:(kq + 1) * 4, :], pb[bk][:, :].rearrange("p (k t) -> p k t", k=4),
                   ['pb%d' % bk], [T('h1T')])
            yield
            bk = bank(0, 2)
            for k in range(8):
                mm(pb[bk][:, 0:E], h1T[q][:, k, :], wrt[:, k, :], k == 0, False, [T('h1T'), 'wrt'], ['pb%d' % bk])
            mm(pb[bk][:, 0:E], ones[0:1, :], brt[0:1, :], False, True, ['ones', 'brt'], ['pb%d' % bk])
            cp('dve', lg[:], pb[bk][:, 0:E], ['pb%d' % bk], [T('lg')])
            yield
            S.op('dve', lambda e: e.max(out=rs_[:, 0:8], in_=lg[:]), [T('lg')], [T('r_mx')])
            yield
            ts('dve', msk[:], lg[:], rs_[:, 3:4], None, ALU.is_ge, None, [T('lg'), T('r_mx')], [T('msk')])
            ts('dve', rs_[:, 8:9], rs_[:, 0:1], -1.0, None, ALU.mult, None, [T('r_mx')], [T('r_nm')])
            yield
            act(exg[:], lg[:], AF.Exp, [T('lg'), T('r_nm')], [T('exg')], bias=rs_[:, 8:9])
            bk = bank(0, 2)
            mm(pb[bk][:, 0:E], tri_excl[:], msk[:], True, True, ['tri_excl', T('msk')], ['pb%d' % bk])
            tt('dve', posf[:], pb[bk][:, 0:E], basecnt[:], ALU.add, ['pb%d' % bk, 'basecnt'], [T('posf')])
            bk = bank(0, 2)
            mm(pb[bk][:, 0:E], ones[:], msk[:], True, True, ['ones', T('msk')], ['pb%d' % bk])
            tt('dve', basecnt[:], basecnt[:], pb[bk][:, 0:E], ALU.add, ['pb%d' % bk, 'basecnt'], ['basecnt'])
            yield
            tt('dve', exg[:], exg[:], msk[:], ALU.mult, [T('exg'), T('msk')], [T('exg')])
            tt('dve', posf[:], posf[:], eoff[:], ALU.add, [T('posf'), 'eoff'], [T('posf')])
            yield
            S.op('dve', lambda e: e.reduce_sum(out=rs_[:, 9:10], in_=exg[:], axis=AX.X), [T('exg')], [T('r_sum')])
            tt('dve', posf[:], posf[:], msk[:], ALU.mult, [T('posf'), T('msk')], [T('posf')])
            yield
            S.op('dve', lambda e: e.reciprocal(out=rs_[:, 10:11], in_=rs_[:, 9:10]), [T('r_sum')], [T('r_rs')])
            S.op('dve', lambda e: e.max(out=rs_[:, 16:24], in_=posf[:]), [T('posf')], [T('r_v8')])
            yield
            ts('dve', exg[:], exg[:], rs_[:, 10:11], None, ALU.mult, None, [T('exg'), T('r_rs')], [T('exg')])
            ts('dve', slots_all[:, xi, :], rs_[:, 16:20], -1.0, None, ALU.add, None, [T('r_v8')], ['slots%d' % xi])
            tt('dve', ohe[:], msk[:], ie1[:], ALU.mult, [T('msk'), 'ie1'], [T('ohe')])
            yield
            for k in range(4):
                tok = 'XS_%d_%d' % (xi, k)
                xs_tokens.append(tok)
                S.dma('pool', lambda e, xi=xi, k=k, q=q: e.indirect_dma_start(
                    out=XS[0:E * C, :], out_offset=bass.IndirectOffsetOnAxis(ap=slots_all[:, xi, k:k + 1], axis=0),
                    in_=h1b[q][:], in_offset=None), [T('h1b'), 'slots%d' % xi] + xs0_tokens, [tok])
            S.op('dve', lambda e: e.max(out=rs_[:, 24:32], in_=ohe[:]), [T('ohe')], [T('r_e8')])
            yield
            for k in range(4):
                ts('dve', ohe[:], ie1[:], rs_[:, 24 + k:25 + k], None, ALU.is_equal, None, ['ie1', T('r_e8')], [T('ohe')])
                yield
                tt('dve', ohe[:], ohe[:], exg[:], ALU.mult, [T('ohe'), T('exg')], [T('ohe')])
                yield
                S.op('dve', lambda e, xi=xi, k=k: e.reduce_sum(out=gates_all[:, xi, k:k + 1], in_=ohe[:], axis=AX.X), [T('ohe')], ['gates%d' % xi])
                yield

        prev_gens = []
        for sq in range(NS):
            for gq in range((NT - 1) // QG):
                ti0 = sq * NT + 1 + gq * QG
                for j in range(QG):
                    dma('sp', yin[:, :, j * 128:(j + 1) * 128], YT[ti0 + j, :, :, :],
                        ['YTa_%d' % (ti0 + j), 'YTm_%d' % (ti0 + j)], ['yin0'])
                    dma('sp', gin[:, :, j * 128:(j + 1) * 128], ZG[ti0 + j, :, :, :], ['ZG_%d' % (ti0 + j)], ['gin0'])
                for dc in range(8):
                    ba = bank(2, 8)
                    for kc in range(4):
                        mm(pb[ba][:, 0:QW], wa[:, kc, dc * 128:(dc + 1) * 128], yin[:, kc, :], kc == 0, kc == 3, ['wa', 'yin0'], ['pb%d' % ba])
                    bm = bank(2, 8)
                    for kc in range(4):
                        mm(pb[bm][:, 0:QW], wm_[:, kc, dc * 128:(dc + 1) * 128], yin[:, 4 + kc, :], kc == 0, kc == 3, ['wm', 'yin0'], ['pb%d' % bm])
                    tb = 2 * (dc % 2)
                    tt('dve', t12[tb][:], pb[ba][:, 0:QW], gin[:, dc, :], ALU.mult, ['pb%d' % ba, 'gin0'], ['t12_%d' % tb])
                    tt('dve', t12[tb + 1][:], pb[bm][:, 0:QW], gin[:, 8 + dc, :], ALU.mult, ['pb%d' % bm, 'gin0'], ['t12_%d' % (tb + 1)])
                    tt('pool', mrgT[:, dc, :], t12[tb][:], t12[tb + 1][:], ALU.add, ['t12_%d' % tb, 't12_%d' % (tb + 1)], ['mrgT'])
                gens = [b2_tile(sq, gq, j) for j in range(QG)]
                live = list(gens)
                while live:
                    for gnr in list(live):
                        try:
                            next(gnr)
                        except StopIteration:
                            live.remove(gnr)
        S.finish()
        S.barrier()
        p2.close()
        pw.close()

        p3 = ExitStack()

        def sb3(name, shape, dt=F32):
            return p3.enter_context(nc.sbuf_tensor(name, list(shape), dt))

        NST = C // 128
        NWB = 8
        wbuf = [sb3("wbuf%d" % i, [128, 8 * 512], BF16) for i in range(NWB)]
        XTL = [sb3("XT%d" % i, [128, 8, C], BF16) for i in range(2)]; actT = sb3("actT", [128, 8, C], BF16)
        xsin = [sb3("xsin%d" % i, [128, D], BF16) for i in range(NST)]
        identc = sb3("identc", [128, 128], BF16)
        cp('dve', identc[:], ident[:], ['ident'], ['identc'])
        ystg = [sb3("ystg%d" % i, [128, D], BF16) for i in range(2)]
        bdb = [sb3("bdb%d" % i, [128, D]) for i in range(2)]
        gtmp = [sb3("gtmp%d" % i, [128, 512]) for i in range(2)]; stmp = [sb3("stmp%d" % i, [128, 512]) for i in range(2)]
        ltmp = [sb3("ltmp%d" % i, [128, 512]) for i in range(2)]
        ci_ = [0]
        bgs = [sb3("bgs%d" % i, [128, 16]) for i in range(2)]
        ones1 = sb3("ones1", [1, 128])
        dma('sp', ones1[:], cst['ones'][0:1, :], [], ['ones1'])
        wi = [0]
        xsc = [0]
        tgs = [(t0, min(512, C - t0)) for t0 in range(0, C, 512)]

        def need(gidx):
            while wi[0] <= min(gidx + 5, E * 6 - 1):
                gp = wi[0]
                wi[0] += 1
                ex_, pi_ = gp // 6, gp % 6
                wb_ = gp % NWB
                if pi_ < 4:
                    dma('pool', wbuf[wb_][:, :].rearrange("p (k c) -> p k c", k=8),
                        w_gu[ex_, :, pi_ * 512:(pi_ + 1) * 512].rearrange("(k p) c -> p k c", p=128), [], ['wbuf%d' % wb_])
                else:
                    dma('pool', wbuf[wb_][:, :].rearrange("p (k c) -> p k c", k=4),
                        w_down[ex_, (pi_ - 4) * 512:(pi_ - 3) * 512, :].rearrange("(k p) c -> p k c", p=128), [], ['wbuf%d' % wb_])
        def prep_gen(ex):
            XTn, xk = XTL[ex % 2], 'XT%d' % (ex % 2)
            for stt_ in range(NST):
                xb = stt_
                bk = bank(0, 2)
                pvb = pb[bk][:, :].bitcast(BF16)
                for k in range(8):
                    S.op('pe', lambda e, k=k, xb=xb, pvb=pvb: e.transpose(pvb[:, k * 128:(k + 1) * 128], xsin[xb][:, k * 128:(k + 1) * 128], identc[:]),
                         ['xsin%d' % xb, 'identc'], ['pb%d' % bk])
                cp('act' if stt_ % 2 == 0 else 'dve', XTn[:, :, stt_ * 128:(stt_ + 1) * 128],
                   pvb.rearrange("p (k t) -> p k t", k=8), ['pb%d' % bk], [xk])
                yield

        def issue_xs(ex_):
            for stt_ in range(NST):
                r0_ = ex_ * C + stt_ * 128
                dma('sp', xsin[stt_][:], XS[r0_:r0_ + 128, :], xs_tokens, ['xsin%d' % stt_])

        issue_xs(0)
        for _ in prep_gen(0):
            pass
        for ex in range(E):
            eb = ex % 2
            XTc, xtk = XTL[ex % 2], 'XT%d' % (ex % 2)
            if ex + 1 < E:
                issue_xs(ex + 1)
            dma('sp', bgs[eb][:], bguh[ex, :, :], [], ['bgs%d' % eb])
            ts('dve', bgs[eb][:, 8:16], bgs[eb][:, 8:16], 1.0, None, ALU.add, None, ['bgs%d' % eb], ['bgs%d' % eb])
            dma('sp', bdb[eb][:], b_down[ex:ex + 1, :].partition_broadcast(128), [], ['bdb%d' % eb])
            wl = [(ex * 6 + pi) % NWB for pi in range(6)]
            need(ex * 6)
            for fc in range(8):
                need(ex * 6 + fc // 2)
                wb = wl[fc // 2]
                wv = wbuf[wb][:, :].rearrange("p (k c) -> p k c", k=8)
                base = (fc % 2) * 256
                for (t0, n) in tgs:
                    bg_ = bank(2, 8)
                    for k in range(8):
                        mm(pb[bg_][:, 0:n], wv[:, k, base:base + 256:2], XTc[:, k, t0:t0 + n], k == 0, k == 7, ['wbuf%d' % wb, xtk], ['pb%d' % bg_])
                    bl_ = bank(2, 8)
                    for k in range(8):
                        mm(pb[bl_][:, 0:n], wv[:, k, base + 1:base + 256:2], XTc[:, k, t0:t0 + n], k == 0, k == 7, ['wbuf%d' % wb, xtk], ['pb%d' % bl_])
                    q_ = ci_[0] % 2
                    ci_[0] += 1
                    G_, S_, L_ = gtmp[q_], stmp[q_], ltmp[q_]
                    gk, sk, lk = 'gtmp%d' % q_, 'stmp%d' % q_, 'ltmp%d' % q_
                    ts('dve', G_[:, 0:n], pb[bg_][:, 0:n], bgs[eb][:, fc:fc + 1], 7.0, ALU.add, ALU.min, ['pb%d' % bg_, 'bgs%d' % eb], [gk])
                    act(S_[:, 0:n], G_[:, 0:n], AF.Sigmoid, [gk], [sk], scale=1.702)
                    ts('dve', L_[:, 0:n], pb[bl_][:, 0:n], bgs[eb][:, 8 + fc:9 + fc], 8.0, ALU.add, ALU.min, ['pb%d' % bl_, 'bgs%d' % eb], [lk])
                    tt('dve', G_[:, 0:n], G_[:, 0:n], S_[:, 0:n], ALU.mult, [gk, sk], [gk])
                    stt(actT[:, fc, t0:t0 + n], L_[:, 0:n], -6.0, G_[:, 0:n], ALU.max, ALU.mult, [lk, gk], ['actT'])
            need(ex * 6 + 5)
            pg = prep_gen(ex + 1) if ex + 1 < E else iter(())
            for stt_ in range(NST):
                yb = stt_ % 2
                for half in range(2):
                    bk = bank(2, 8)
                    for fc in range(8):
                        wb = wl[4 + fc // 4]
                        wv = wbuf[wb][:, :].rearrange("p (k c) -> p k c", k=4)
                        mm(pb[bk][:, :], actT[:, fc, stt_ * 128:(stt_ + 1) * 128], wv[:, fc % 4, half * 512:(half + 1) * 512], fc == 0, fc == 7,
                           ['actT', 'wbuf%d' % wb], ['pb%d' % bk])
                    tt('dve', ystg[yb][:, half * 512:(half + 1) * 512], pb[bk][:, :], bdb[eb][:, half * 512:(half + 1) * 512], ALU.add,
                       ['pb%d' % bk, 'bdb%d' % eb], ['ystg%d' % yb])
                r0 = ex * C + stt_ * 128
                dma('sp', YS[r0:r0 + 128, :], ystg[yb][:], ['ystg%d' % yb], ['YS_%d_%d' % (ex, stt_)])
                next(pg, None)
            for _ in pg:
                pass
        S.finish()
        S.barrier()
        p3.close()

        p4 = ExitStack()

        def sb4(name, shape, dt=F32):
            return p4.enter_context(nc.sbuf_tensor(name, list(shape), dt))

        load_ln(sb4, 4)
        ys_tokens = ['YS_%d_%d' % (ex, q) for ex in range(E) for q in range(NST)]
        ND = 4
        yk = [[sb4("yk%d_%d" % (i, k), [128, D], BF16) for k in range(4)] for i in range(ND)]
        h1in = [sb4("h1in%d" % i, [128, D]) for i in range(ND)]
        accd = [sb4("accd%d" % i, [128, D]) for i in range(2)]
        lntmp4 = [sb4("lntmp4_%d" % i, [128, D]) for i in range(2)]; lnst4 = [sb4("lnst4_%d" % i, [128, 16]) for i in range(2)]
        od = [sb4("od%d" % i, [128, D]) for i in range(ND)]
        for xi in range(NXT):
            b2 = xi % ND
            a2 = xi % 2
            ac, ak = accd[a2], 'accd%d' % a2
            dma('sp', h1in[b2][:], H1[xi * 128:(xi + 1) * 128, :], ['H1_%d' % xi], ['h1in%d' % b2])
            for k in range(4):
                S.dma('pool', lambda e, xi=xi, k=k, b2=b2: e.indirect_dma_start(
                    out=yk[b2][k][:], out_offset=None, in_=YS[0:E * C, :],
                    in_offset=bass.IndirectOffsetOnAxis(ap=slots_all[:, xi, k:k + 1], axis=0)),
                    ys_tokens + ['slots%d' % xi], ['yk%d_%d' % (b2, k)])
            act(ac[:], yk[b2][0][:], AF.Copy, ['yk%d_0' % b2, 'gates%d' % xi], [ak], scale=gates_all[:, xi, 0:1])
            for k in range(1, 4):
                stt(ac[:], yk[b2][k][:], gates_all[:, xi, k:k + 1], ac[:], ALU.mult, ALU.add, ['yk%d_%d' % (b2, k), 'gates%d' % xi, ak], [ak])
            stt(ac[:], h1in[b2][:], ALPHA, ac[:], ALU.mult, ALU.add, ['h1in%d' % b2, ak], [ak])
            layer_norm(ac[:], od[b2][:], 4, [ak], ['od%d' % b2], 'ln2_%d' % a2, lntmp4[a2][:], lnst4[a2], 'lntmp4_%d' % a2)
            dma('sp', out[xi * 128:(xi + 1) * 128, :], od[b2][:], ['od%d' % b2], ['out_%d' % xi])
        finals = [v for k, v in S.last_w.items() if k.startswith('out_') or (dbg and k.startswith(('H0_', 'ZTM_', 'ZFM_', 'H1_')))]
        S.finish(finals)
        p4.close()
    return nc


NCORES = 8
CFG = dict(NS=2, NT=33, GT=11, E=32, C=1280)


def _core_inputs(inp, seqs, NT, E, C):
    f = lambda a: np.ascontiguousarray(a, dtype=np.float32)
    x = inp['x']; meta = inp['meta']
    rows = []
    for b in seqs:
        rows += [np.zeros((112, D), np.float32), meta, x[b]]
    m = {'xin': np.concatenate(rows, 0)}
    m['w_in'] = inp['w_in'][0]
    m['cwb'] = np.concatenate([inp['conv_w'][0].reshape(4, 8, 128).transpose(2, 1, 0).reshape(128, 32),
                               inp['conv_b'][0].reshape(8, 128).T], axis=1)
    m['gate_bias'] = inp['gate_bias'][0][None]
    m['lamv'] = np.concatenate([inp['lam_q1'][0], inp['lam_k1'][0], inp['lam_q2'][0], inp['lam_k2'][0]])[None]
    m['att_norm_g'] = inp['att_norm_g'][0][None]; m['ml_norm_g'] = inp['ml_norm_g'][0][None]
    m['w_att_out'] = inp['w_att_out'][0]; m['w_ml_out'] = inp['w_ml_out'][0]; m['w_o'] = inp['w_o'][0]
    m['lnp'] = np.stack([inp['emb_ln_g'], inp['emb_ln_b'], inp['ln1_g'][0], inp['ln1_b'][0], inp['ln2_g'][0], inp['ln2_b'][0]])
    m['w_router'] = inp['w_router'][0]; m['b_router'] = inp['b_router'][0][None]
    m['w_gu'] = inp['w_gu'][0]; m['w_down'] = inp['w_down'][0]; m['b_down'] = inp['b_down'][0]
    bg = inp['b_gu'][0]
    m['bguh'] = np.concatenate([bg[:, 0::2].reshape(E, 8, 128).transpose(0, 2, 1), bg[:, 1::2].reshape(E, 8, 128).transpose(0, 2, 1)], axis=2)
    m['b_gu'] = bg
    for k, v in host_consts(NT).items():
        m['c_' + k] = v
    m['ie1'] = np.tile((np.arange(E) + 1).astype(np.float32)[None], (128, 1))
    m['eoff'] = np.tile((np.arange(E) * C + 1).astype(np.float32)[None], (128, 1))
    return {k: f(v) for k, v in m.items()}


def kernel(**inputs):
    inp = {k: np.asarray(v) for k, v in inputs.items()}
    cfg = CFG
    nc = build(cfg['NS'], cfg['NT'], cfg['GT'], cfg['E'], cfg['C'])
    shared = None
    in_maps = []
    for c in range(NCORES):
        seqs = [c * cfg['NS'] + i for i in range(cfg['NS'])]
        if shared is None:
            shared = _core_inputs(inp, seqs, cfg['NT'], cfg['E'], cfg['C'])
            in_maps.append(shared)
        else:
            m = dict(shared)
            rows = []
            for b in seqs:
                rows += [np.zeros((112, D), np.float32), inp['meta'].astype(np.float32), inp['x'][b].astype(np.float32)]
            m['xin'] = np.ascontiguousarray(np.concatenate(rows, 0))
            in_maps.append(m)
    res = run_bass_kernel_spmd(nc, in_maps, core_ids=list(range(NCORES)))
    S_ = inp['x'].shape[1]
    outs = [np.asarray(r['out']).reshape(cfg['NS'], S_, D) for r in res.results]
    return np.concatenate(outs, axis=0).astype(np.float32)
```

```python
import math
from contextlib import ExitStack
import numpy as np
import concourse.bass as bass
import concourse.mybir as mybir
from concourse.bass_utils import run_bass_kernel_spmd

F32 = mybir.dt.float32
F32R = mybir.dt.float32r
I32 = mybir.dt.int32
BF16 = mybir.dt.bfloat16
AF = mybir.ActivationFunctionType
ALU = mybir.AluOpType
AX = mybir.AxisListType

ENGS = ('pe', 'act', 'dve', 'pool', 'sp')
D = 1024
NIN = 5640
NEG = -1e30
EPS = 1e-5
ALPHA = 2.0 ** 0.25
LAMBDA_INIT = 0.8 - 0.6 * math.exp(0.0)


class Sched:
    def __init__(self, nc, stack, n_dma_sems=48):
        self.nc = nc
        self.ops = {e: [] for e in ENGS}
        self.sem = {e: stack.enter_context(nc.semaphore('s_' + e)) for e in ENGS}
        self.cnt = {e: 0 for e in ENGS}
        self.waited = {e: {} for e in ENGS}
        self.last_w = {}
        self.readers = {}
        self.dsem = [stack.enter_context(nc.semaphore('d%d' % i)) for i in range(n_dma_sems)]
        self.dcnt = [0] * n_dma_sems
        self.drr = 0
        self.drr_sw = 0

    def _deps(self, reads, writes):
        deps = set()
        for t in reads:
            if t in self.last_w:
                deps.add(self.last_w[t])
        for t in writes:
            if t in self.last_w:
                deps.add(self.last_w[t])
            for r in self.readers.get(t, ()):
                deps.add(r)
        return deps

    def _emit_waits(self, eng, deps, is_dma):
        best = {}
        for (k, v) in deps:
            if best.get(k, 0) < v:
                best[k] = v
        waits = []
        for k, v in best.items():
            if k == 'pe' and eng == 'pe' and not is_dma:
                continue
            if self.waited[eng].get(k, 0) >= v:
                continue
            self.waited[eng][k] = v
            waits.append((k, v))
        return waits

    def _semof(self, k):
        return self.sem[k] if isinstance(k, str) else self.dsem[k]

    def _record(self, me, reads, writes):
        for t in writes:
            self.last_w[t] = me
            self.readers[t] = []
        for t in reads:
            self.readers.setdefault(t, []).append(me)

    def barrier(self):
        deps = set((e, self.cnt[e]) for e in ENGS if self.cnt[e] > 0)
        deps |= set((i, v) for i, v in enumerate(self.dcnt) if v > 0)
        self.need_barrier = {e: set(deps) for e in ENGS}

    def _deps_b(self, eng, reads, writes):
        deps = self._deps(reads, writes)
        nb = getattr(self, 'need_barrier', {}).pop(eng, None)
        if nb:
            deps |= nb
        return deps

    def op(self, eng, fn, reads=(), writes=()):
        waits = self._emit_waits(eng, self._deps_b(eng, reads, writes), False)
        self.cnt[eng] += 1
        me = (eng, self.cnt[eng])
        self.ops[eng].append((waits, fn, (eng, 1)))
        self._record(me, reads, writes)
        return me

    def dma(self, eng, fn, reads=(), writes=()):
        deps = self._deps_b(eng, reads, writes)
        nsw = 16
        if eng == 'pool':
            si = self.drr_sw
            self.drr_sw = (self.drr_sw + 1) % nsw
        else:
            si = nsw + self.drr
            self.drr = (self.drr + 1) % (len(self.dsem) - nsw)
        if self.dcnt[si] > 0:
            deps.add((si, self.dcnt[si]))
        waits = self._emit_waits(eng, deps, True)
        self.dcnt[si] += 16
        me = (si, self.dcnt[si])
        self.ops[eng].append((waits, fn, (si, 16)))
        self._record(me, reads, writes)
        return me

    def finish(self, final_deps=()):
        nc = self.nc
        engmap = {'pe': 'tensor', 'act': 'scalar', 'dve': 'vector', 'pool': 'gpsimd', 'sp': 'sync'}
        sched = self
        fw = self._emit_waits('sp', set(final_deps), True)
        with nc.Block() as block:
            for e in ENGS:
                def body(engine, e=e):
                    for (waits, fn, (sk, inc)) in sched.ops[e]:
                        for (k, v) in waits:
                            engine.wait_ge(sched._semof(k), v)
                        fn(engine).then_inc(sched._semof(sk), inc)
                    if e == 'sp':
                        for (k, v) in fw:
                            engine.wait_ge(sched._semof(k), v)
                getattr(block, engmap[e])(body)
        self.ops = {e: [] for e in ENGS}


TM_PIECES = [(0, 512), (512, 1024), (1024, 1536), (2560, 3072), (3072, 3584), (3584, 3592)]
TM_OFF = [0, 512, 1024, 1536, 2048, 2560]
ZTM_W = 2568
FM_PIECES = [(1536, 2048), (2048, 2560), (3592, 4104), (4104, 4616), (4616, 5128), (5128, 5640)]


def host_consts(NT):
    c = {}
    c['ident'] = np.eye(128, dtype=np.float32)
    r = np.arange(128)
    c['tri_incl'] = (r[:, None] <= r[None, :]).astype(np.float32)
    c['tri_excl'] = (r[:, None] < r[None, :]).astype(np.float32)
    c['cmask'] = np.where(r[None, :] <= r[:, None], 0.0, NEG).astype(np.float32)
    c['cmaskT'] = np.ascontiguousarray(c['cmask'].T)
    sel = np.zeros((128, 128), np.float32); sel[127, :] = 1.0
    c['sel127'] = sel
    selh = np.zeros((4, 4, 128), np.float32)
    for h in range(4):
        selh[h, h, :] = 1.0
    c['selh'] = selh.reshape(4, 512)
    c['ones'] = np.ones((128, 128), np.float32)
    pos = (np.arange(NT * 128) - 112).astype(np.float32)
    inv = (10000.0 ** (-np.arange(32, dtype=np.float32) / 32)).astype(np.float32)
    ang = pos[:, None] * inv[None, :]
    c['cs64'] = np.concatenate([np.cos(ang), np.sin(ang)], axis=1).astype(np.float32)
    pv = np.zeros((128, 2), np.float32)
    pv[112:, 0] = 1.0
    pv[:112, 1] = NEG
    c['padv'] = pv
    return c


def build(NS, NT, GT, E, C, dbg=False):
    nc = bass.Bass("TRN2", target_bir_lowering=False)
    NTILE = NS * NT
    NTOK = NTILE * 128
    NXT = NS * (NT - 1)
    NG = NTILE // GT
    GW = GT * 128
    assert NT % GT == 0

    def din(name, shape, dt=F32):
        return nc.dram_tensor(name, list(shape), dt, kind="ExternalInput").ap()

    def dscr(name, shape, dt=F32):
        return nc.dram_tensor(name, list(shape), dt, kind="Internal").ap()

    xin = din("xin", [NTOK, D])
    w_in = din("w_in", [D, NIN])
    cwb_d = din("cwb", [128, 40])
    gate_bias = din("gate_bias", [1, 8])
    lamv = din("lamv", [1, 256])
    att_norm_g = din("att_norm_g", [1, 128]); ml_norm_g = din("ml_norm_g", [1, 512])
    w_att_out = din("w_att_out", [512, D]); w_ml_out = din("w_ml_out", [512, D]); w_o = din("w_o", [D, D])
    lnp = din("lnp", [6, D])
    w_router = din("w_router", [D, E]); b_router = din("b_router", [1, E])
    w_gu = din("w_gu", [E, D, 2048]); b_gu = din("b_gu", [E, 2048])
    w_down = din("w_down", [E, D, D]); b_down = din("b_down", [E, D])
    cst = {k: din("c_" + k, v.shape) for k, v in host_consts(NT).items()}
    eoff_d = din("eoff", [128, E]); ie1_d = din("ie1", [128, E])
    bguh = din("bguh", [E, 128, 16])

    out = nc.dram_tensor("out", [NXT * 128, D], F32, kind="ExternalOutput").ap()
    mk = (lambda n, s: nc.dram_tensor(n, list(s), F32, kind="ExternalOutput").ap()) if dbg else dscr
    H0 = mk("H0", [NTOK, D])
    ZTM = mk("ZTM", [NTOK, 1032])
    ZQKV = nc.dram_tensor("ZQKV", [NTOK, 1536], BF16, kind="Internal").ap()
    ZFM = mk("ZFM", [NTILE, 128, 8, 128])
    ZG = nc.dram_tensor("ZG", [NTILE, 128, 16, 128], BF16, kind="Internal").ap()
    YT = nc.dram_tensor("YT", [NTILE, 128, 8, 128], BF16, kind="Internal").ap()
    H1 = mk("H1", [NXT * 128, D])
    XS = nc.dram_tensor("XS", [E * C + 256, D], BF16, kind="Internal").ap()
    YS = nc.dram_tensor("YS", [E * C + 256, D], BF16, kind="Internal").ap()

    with ExitStack() as st:
        S = Sched(nc, st)

        def sb(name, shape, dt=F32):
            return st.enter_context(nc.sbuf_tensor(name, list(shape), dt))

        pb = [st.enter_context(nc.psum_tensor("pb%d" % i, [128, 512], F32)) for i in range(8)]
        prr = [0]

        def bank(lo=0, hi=8):
            i = lo + prr[0] % (hi - lo)
            prr[0] += 1
            return i

        def dma(eng, out_ap, in_ap, reads, writes, **kw):
            return S.dma(eng, lambda e: e.dma_start(out=out_ap, in_=in_ap, **kw), reads, writes)

        def mm(out_ap, lhsT, rhs, start, stop, reads, writes):
            return S.op('pe', lambda e: e.matmul(out_ap, lhsT=lhsT, rhs=rhs, start=start, stop=stop), reads, writes)

        def tr(out_ap, in_ap, reads, writes, ident_ap=None):
            idn = ident[:] if ident_ap is None else ident_ap
            return S.op('pe', lambda e: e.transpose(out_ap, in_ap, idn), list(reads) + ['ident'], writes)

        def act(out_ap, in_ap, func, reads, writes, bias=0.0, scale=1.0, accum=None):
            return S.op('act', lambda e: e.activation(out=out_ap, in_=in_ap, func=func, bias=bias, scale=scale,
                                                      **({'accum_out': accum} if accum is not None else {})),
                        reads, writes)

        def tt(eng, out_ap, a, b, op, reads, writes):
            return S.op(eng, lambda e: e.tensor_tensor(out=out_ap, in0=a, in1=b, op=op), reads, writes)

        def ts(eng, out_ap, a, s1, s2, op0, op1, reads, writes, accum=None):
            if op1 is None:
                return S.op(eng, lambda e: e.tensor_scalar(out=out_ap, in0=a, scalar1=s1, scalar2=None, op0=op0), reads, writes)
            return S.op(eng, lambda e: e.tensor_scalar(out=out_ap, in0=a, scalar1=s1, scalar2=s2, op0=op0, op1=op1,
                                                       **({'accum_out': accum} if accum is not None else {})),
                        reads, writes)

        def stt(out_ap, a, s, b, op0, op1, reads, writes):
            return S.op('dve', lambda e: e.scalar_tensor_tensor(out=out_ap, in0=a, scalar=s, in1=b, op0=op0, op1=op1),
                        reads, writes)

        def cp(eng, out_ap, in_ap, reads, writes):
            if eng == 'act':
                return S.op('act', lambda e: e.copy(out=out_ap, in_=in_ap), reads, writes)
            return S.op(eng, lambda e: e.tensor_copy(out=out_ap, in_=in_ap), reads, writes)

        ident = sb("ident", [128, 128])
        dma('sp', ident[:], cst['ident'][:, :], [], ['ident'])
        lnbh = [None]

        def load_ln(alloc, gi):
            lnbh[0] = alloc("lnb_%d" % gi, [128, 6, D]) if False else alloc("lnb_%d" % gi, [128, 2, D])
            for i in range(2):
                dma('sp', lnbh[0][:, i, :], lnp[gi + i:gi + i + 1, :].partition_broadcast(128), [], ['lnb%d' % (gi + i)])

        def layer_norm(x_ap, y_ap, gi, rd, wr, tag, tmp, stat, tmptok):
            for c2 in range(2):
                S.op('dve', lambda e, c2=c2: e.bn_stats(out=stat[:, c2 * 6:(c2 + 1) * 6], in_=x_ap[:, c2 * 512:(c2 + 1) * 512]),
                     rd, [tag + 'st%d' % c2])
            S.op('dve', lambda e: e.bn_aggr(out=stat[:, 12:14], in_=stat[:, 0:12].rearrange("p (c s) -> p c s", s=6)),
                 [tag + 'st0', tag + 'st1'], [tag + 'ag'])
            act(stat[:, 14:15], stat[:, 13:14], AF.Sqrt, [tag + 'ag'], [tag + 'sd'], bias=epsb[:, 0:1])
            S.op('dve', lambda e: e.reciprocal(out=stat[:, 15:16], in_=stat[:, 14:15]), [tag + 'sd'], [tag + 'rs'])
            stt(tmp, x_ap, stat[:, 12:13], lnbh[0][:, 0, :], ALU.subtract, ALU.mult,
                list(rd) + [tag + 'ag', 'lnb%d' % gi], [tmptok])
            stt(y_ap, tmp, stat[:, 15:16], lnbh[0][:, 1, :], ALU.mult, ALU.add, [tmptok, tag + 'rs', 'lnb%d' % (gi + 1)], wr)

        epsb = sb("epsb", [128, 1])
        S.op('dve', lambda e: e.memset(epsb[:], EPS), [], ['epsb'])

        pa = ExitStack()

        def sba(name, shape, dt=F32):
            return pa.enter_context(nc.sbuf_tensor(name, list(shape), dt))

        load_ln(sba, 0)
        h0T = sba("h0T", [128, 8, GW], F32R)
        wpc = [sba("wpc%d" % i, [128, 8, 512], F32R) for i in range(2)]
        xt = [sba("xt%d" % i, [128, D]) for i in range(4)]
        h0t = [sba("h0t%d" % i, [128, D]) for i in range(4)]
        lntmp = [sba("lntmp%d" % i, [128, D]) for i in range(4)]
        lnst = [sba("lnst%d" % i, [128, 16]) for i in range(4)]
        zst = [sba("zst%d" % i, [128, 512]) for i in range(3)]
        zsb = [sba("zsb%d" % i, [128, 512], BF16) for i in range(3)]
        rt = [sba("rt%d" % i, [128, 256]) for i in range(8)]
        csr = sba("csr", [128, NT, 64])
        for t_ in range(NT):
            dma('sp', csr[:, t_, :], cst['cs64'][t_ * 128:(t_ + 1) * 128, :], [], ['csr'])
        zcs = [sba("zc%d" % i, [128, 3 + GW]) for i in range(2)]
        caccs = [sba("cacc%d" % i, [128, GW]) for i in range(2)]
        fst = [sba("fst%d" % i, [128, GW]) for i in range(2)]
        fstg = [sba("fstg%d" % i, [128, GW], BF16) for i in range(2)]
        hist = sba("hist", [128, 8, 3])
        cw = sba("cw", [128, 8, 4]); cb = sba("cb", [128, 8])
        dma('sp', cw[:], cwb_d[:, 0:32].rearrange("p (c k) -> p c k", k=4), [], ['cw'])
        dma('sp', cb[:], cwb_d[:, 32:40], [], ['cb'])
        pieces = [('tm', i) for i in range(6)] + [('fm', i) for i in range(6)]
        zi = [0]
        pidx = [0]
        allp = [p for _ in range(NG) for p in pieces]

        def load_piece(gp):
            if gp >= len(allp):
                return
            kind_, pi_ = allp[gp]
            a_, b_ = (TM_PIECES if kind_ == 'tm' else FM_PIECES)[pi_]
            dma('pool', wpc[gp % 2][:, :, 0:b_ - a_], w_in[:, a_:b_].rearrange("(k p) c -> p k c", p=128), [], ['wpc%d' % (gp % 2)])

        def tm_tile(pi, g, j, wb):
            c0, c1 = TM_PIECES[pi]
            wcols = c1 - c0
            ti = g * GT + j
            bk = bank(2, 8)
            for k in range(8):
                mm(pb[bk][:, 0:wcols], h0T[:, k, j * 128:(j + 1) * 128], wpc[wb][:, k, 0:wcols], k == 0, k == 7,
                   ['h0T_%d' % j, 'wpc%d' % wb], ['pb%d' % bk])
            zb = zi[0] % 3
            zi[0] += 1
            if pi in (0, 1):
                tl = ti % NT
                zv = pb[bk][:, :].rearrange("p (g h i) -> p g h i", g=8, h=2)
                ov = zsb[zb][:, :].rearrange("p (g h i) -> p g h i", g=8, h=2)
                cv = csr[:, tl, 0:32].unsqueeze(1).to_broadcast([128, 8, 32])
                sv = csr[:, tl, 32:64].unsqueeze(1).to_broadcast([128, 8, 32])
                ro = 4 * (rtc[0] % 2)
                rtc[0] += 1
                r4 = [rt[ro + q][:, :].rearrange("p (g i) -> p g i", g=8) for q in range(4)]
                rk = ['rt%d' % (ro + q) for q in range(4)]
                rd = ['pb%d' % bk, 'csr']
                tt('dve', r4[0], zv[:, :, 0, :], cv, ALU.mult, rd, [rk[0]])
                tt('dve', r4[1], zv[:, :, 1, :], sv, ALU.mult, rd, [rk[1]])
                tt('dve', r4[2], zv[:, :, 1, :], cv, ALU.mult, rd, [rk[2]])
                tt('dve', r4[3], zv[:, :, 0, :], sv, ALU.mult, rd, [rk[3]])
                tt('pool', ov[:, :, 0, :], r4[0], r4[1], ALU.subtract, [rk[0], rk[1]], ['zsb%d' % zb])
                tt('pool', ov[:, :, 1, :], r4[2], r4[3], ALU.add, [rk[2], rk[3]], ['zsb%d' % zb])
            elif pi == 2:
                cp('act', zsb[zb][:, 0:wcols], pb[bk][:, 0:wcols], ['pb%d' % bk], ['zsb%d' % zb])
            else:
                cp('act', zst[zb][:, 0:wcols], pb[bk][:, 0:wcols], ['pb%d' % bk], ['zst%d' % zb])
            if pi <= 2:
                dma('sp', ZQKV[ti * 128:(ti + 1) * 128, pi * 512:pi * 512 + wcols], zsb[zb][:, 0:wcols],
                    ['zsb%d' % zb], ['ZQ%d_%d' % (pi, ti)])
            else:
                dma('sp', ZTM[ti * 128:(ti + 1) * 128, TM_OFF[pi] - 1536:TM_OFF[pi] - 1536 + wcols], zst[zb][:, 0:wcols],
                    ['zst%d' % zb], ['ZTM_%d' % ti])

        rtc = [0]
        h0T_all = ['h0T_%d' % j for j in range(GT)]
        for g in range(NG):
            if pidx[0] == 0:
                load_piece(0)
            wb0 = pidx[0] % 2
            load_piece(pidx[0] + 1)
            def a_tile(j):
                ti = g * GT + j
                q = j % 4
                T = lambda nm: '%s%d' % (nm, q)
                x_ap, stat, tmp = xt[q][:], lnst[q], lntmp[q][:]
                dma('sp', xt[q][:], xin[ti * 128:(ti + 1) * 128, :], [], [T('xt')])
                yield
                for c2 in range(2):
                    S.op('dve', lambda e, c2=c2: e.bn_stats(out=stat[:, c2 * 6:(c2 + 1) * 6], in_=x_ap[:, c2 * 512:(c2 + 1) * 512]),
                         [T('xt')], [T('lnAst%d_' % c2)])
                yield
                S.op('dve', lambda e: e.bn_aggr(out=stat[:, 12:14], in_=stat[:, 0:12].rearrange("p (c s) -> p c s", s=6)),
                     [T('lnAst0_'), T('lnAst1_')], [T('lnAag')])
                yield
                act(stat[:, 14:15], stat[:, 13:14], AF.Sqrt, [T('lnAag')], [T('lnAsd')], bias=epsb[:, 0:1])
                stt(tmp, x_ap, stat[:, 12:13], lnbh[0][:, 0, :], ALU.subtract, ALU.mult, [T('xt'), T('lnAag'), 'lnb0'], [T('lntmp')])
                yield
                S.op('dve', lambda e: e.reciprocal(out=stat[:, 15:16], in_=stat[:, 14:15]), [T('lnAsd')], [T('lnArs')])
                yield
                stt(h0t[q][:], tmp, stat[:, 15:16], lnbh[0][:, 1, :], ALU.mult, ALU.add, [T('lntmp'), T('lnArs'), 'lnb1'], [T('h0t')])
                if ti % NT == 0:
                    S.op('pool', lambda e: e.memset(h0t[q][0:112, :], 0.0), [], [T('h0t')])
                yield
                dma('sp', H0[ti * 128:(ti + 1) * 128, :], h0t[q][:], [T('h0t')], ['H0_%d' % ti])
                for kq in range(2):
                    bk = bank(0, 2)
                    for k4 in range(4):
                        k = kq * 4 + k4
                        tr(pb[bk][:, k4 * 128:(k4 + 1) * 128], h0t[q][:, k * 128:(k + 1) * 128], [T('h0t')], ['pb%d' % bk])
                    cp('act' if kq == 0 else 'dve', h0T[:, kq * 4:(kq + 1) * 4, j * 128:(j + 1) * 128],
                       pb[bk][:, :].rearrange("p (k t) -> p k t", k=4), ['pb%d' % bk], ['h0T_%d' % j])
                    yield
                tm_tile(0, g, j, wb0)
                yield

            live = []
            nxt = 0
            while nxt < GT or live:
                while nxt < GT and len(live) < 4:
                    live.append(a_tile(nxt))
                    nxt += 1
                for gnr in list(live):
                    try:
                        next(gnr)
                    except StopIteration:
                        live.remove(gnr)
            for pidx_local, (kind, pi) in enumerate(pieces):
                c0, c1 = (TM_PIECES if kind == 'tm' else FM_PIECES)[pi]
                wcols = c1 - c0
                wb = pidx[0] % 2
                pidx[0] += 1
                if pidx_local == 0:
                    continue
                load_piece(pidx[0])
                if kind == 'tm':
                    for j in range(GT):
                        tm_tile(pi, g, j, wb)
                else:
                    for cc in range(4):
                        fc = pi * 4 + cc
                        fb = fc % 2
                        zc = zcs[fb]; cacc = caccs[fb]; zck = 'zc%d' % fb; cak = 'cacc%d' % fb
                        nsub = (GW + 511) // 512
                        for sg in range(nsub):
                            t0 = sg * 512
                            n = min(512, GW - t0)
                            bk = bank(2, 8)
                            for k in range(8):
                                mm(pb[bk][:, 0:n], wpc[wb][:, k, cc * 128:(cc + 1) * 128], h0T[:, k, t0:t0 + n], k == 0, k == 7,
                                   h0T_all + ['wpc%d' % wb], ['pb%d' % bk])
                            if fc < 8:
                                cp('act', zc[:, 3 + t0:3 + t0 + n], pb[bk][:, 0:n], ['pb%d' % bk], [zck])
                            else:
                                act(fstg[fb][:, t0:t0 + n], pb[bk][:, 0:n], AF.Sigmoid, ['pb%d' % bk], ['fstg%d' % fb])
                        if fc < 8:
                            if (g * GT) % NT == 0:
                                S.op('pool', lambda e, zc=zc: e.memset(zc[:, 0:3], 0.0), [], [zck])
                            else:
                                cp('pool', zc[:, 0:3], hist[:, fc, :], ['hist%d' % fc], [zck])
                            ts('dve', cacc[:, :], zc[:, 3:3 + GW], cw[:, fc, 3:4], cb[:, fc:fc + 1], ALU.mult, ALU.add,
                               [zck, 'cw', 'cb'], [cak])
                            for tap in range(3):
                                stt(cacc[:, :], zc[:, tap:tap + GW], cw[:, fc, tap:tap + 1], cacc[:, :], ALU.mult, ALU.add,
                                    [zck, 'cw', cak], [cak])
                            cp('pool', hist[:, fc, :], zc[:, GW:GW + 3], [zck], ['hist%d' % fc])
                            act(fst[fb][:, :], cacc[:, :], AF.Silu, [cak], ['fst%d' % fb],
                                )
                            if fc >= 4:
                                ts('dve', fst[fb][:, :], fst[fb][:, :], 128.0 ** -0.5, None, ALU.mult, None, ['fst%d' % fb], ['fst%d' % fb])
                        if fc < 8:
                            dma('sp', ZFM[g * GT:(g + 1) * GT, :, fc, :].rearrange("j p t -> p j t"),
                                fst[fb][:, :].rearrange("p (j t) -> p j t", j=GT), ['fst%d' % fb],
                                ['ZFM_%d' % t_ for t_ in range(g * GT, (g + 1) * GT)])
                        else:
                            dma('sp', ZG[g * GT:(g + 1) * GT, :, fc - 8, :].rearrange("j p t -> p j t"),
                                fstg[fb][:, :].rearrange("p (j t) -> p j t", j=GT), ['fstg%d' % fb],
                                ['ZG_%d' % t_ for t_ in range(g * GT, (g + 1) * GT)])
        S.finish()
        S.barrier()
        pa.close()

        gates_all = sb("gates_all", [128, NXT, 4]); slots_all = sb("slots_all", [128, NXT, 4], I32)
        pw = ExitStack()
        wa = pw.enter_context(nc.sbuf_tensor("wa", [128, 4, D], BF16)); wm_ = pw.enter_context(nc.sbuf_tensor("wm", [128, 4, D], BF16))
        wo = pw.enter_context(nc.sbuf_tensor("wo", [128, 8, D], BF16))
        dma('pool', wa[:], w_att_out.rearrange("(k p) c -> p k c", p=128), [], ['wa'])
        dma('pool', wm_[:], w_ml_out.rearrange("(k p) c -> p k c", p=128), [], ['wm'])
        for k2 in range(2):
            dma('pool', wo[:, k2 * 4:(k2 + 1) * 4, :], w_o[k2 * 512:(k2 + 1) * 512, :].rearrange("(k p) c -> p k c", p=128), [], ['wo'])
        p1 = ExitStack()

        def sb1(name, shape, dt=F32):
            return p1.enter_context(nc.sbuf_tensor(name, list(shape), dt))

        KMAX = 16 + (NT - 1) * 128
        KT = sb1("KT", [128, 4, KMAX], BF16)
        Vc = sb1("Vc", [128, NT, 4, 129], BF16)
        qkin = [sb1("qkin%d" % i, [128, 1024], BF16) for i in range(2)]
        identb = sb1("identb", [128, 128], BF16)
        cp('dve', identb[:], ident[:], ['ident'], ['identb'])
        qT = sb1("qT", [128, 4, 128], BF16)
        PTb = [sb1("PTb%d" % i, [128, 512], BF16) for i in range(6)]
        cmaskT = sb1("cmaskT", [128, 128]); onesb1 = sb1("onesb1", [8, 128]); sqt = sb1("sqt", [128, 512])
        nrm = sb1("nrm", [128, 32]); negc = sb1("negc", [128, 8]); kmx = sb1("kmx", [8, 8]); dg8 = sb1("dg8", [8, 8])
        osb = sb1("osb", [128, 512]); otmp = sb1("otmp", [128, 512])
        mxc = sb1("mxc", [128, 8, 16]); smc = sb1("smc", [128, 8, 16])
        att_s = sb1("att_s", [128, 64])
        cmask = sb1("cmask", [128, 128])
        dma('sp', cmask[:], cst['cmask'][:, :], [], ['cmask'])
        dma('sp', cmaskT[:], cst['cmaskT'][:, :], [], ['cmaskT'])
        dma('sp', onesb1[:], cst['ones'][0:8, :], [], ['onesb1'])
        S.op('pool', lambda e: e.memset(Vc[:, :, :, 128:129], 1.0), [], ['Vones'])
        lamb = sb1("lamb", [128, 256]); lams = sb1("lams", [128, 8])
        dma('sp', lamb[:], lamv.partition_broadcast(128), [], ['lamb'])
        tt('dve', lamb[:, 0:64], lamb[:, 0:64], lamb[:, 64:128], ALU.mult, ['lamb'], ['lamb'])
        tt('dve', lamb[:, 128:192], lamb[:, 128:192], lamb[:, 192:256], ALU.mult, ['lamb'], ['lamb'])
        S.op('dve', lambda e: e.reduce_sum(out=lams[:, 0:1], in_=lamb[:, 0:64], axis=AX.X), ['lamb'], ['lams'])
        S.op('dve', lambda e: e.reduce_sum(out=lams[:, 1:2], in_=lamb[:, 128:192], axis=AX.X), ['lamb'], ['lams'])
        act(lams[:, 2:4], lams[:, 0:2], AF.Exp, ['lams'], ['lams'])
        tt('dve', lams[:, 4:5], lams[:, 3:4], lams[:, 2:3], ALU.subtract, ['lams'], ['lams'])
        ts('dve', lams[:, 5:6], lams[:, 4:5], -LAMBDA_INIT, None, ALU.add, None, ['lams'], ['lams'])
        gatt = sb1("gatt", [128, 128])
        dma('sp', gatt[:], att_norm_g.partition_broadcast(128), [], ['gatt'])
        ts('dve', gatt[:], gatt[:], 1.0 - LAMBDA_INIT, None, ALU.mult, None, ['gatt'], ['gatt'])
        yatt = sb1("yatt", [128, 512])
        ytile = [sb1("ytile%d" % i, [128, 8, 128], BF16) for i in range(2)]

        tri_incl = sb1("tri_incl", [128, 128]); sel127 = sb1("sel127", [128, 128]); selh = sb1("selh", [4, 512])
        padv = sb1("padv", [128, 2]); gbias = sb1("gbias", [128, 8]); mlg = sb1("mlg", [128, 512])
        dma('sp', tri_incl[:], cst['tri_incl'][:, :], [], ['tri_incl'])
        dma('sp', sel127[:], cst['sel127'][:, :], [], ['sel127'])
        dma('sp', selh[:], cst['selh'][:, :], [], ['selh'])
        dma('sp', padv[:], cst['padv'][:, :], [], ['padv'])
        dma('sp', gbias[:], gate_bias.partition_broadcast(128), [], ['gbias'])
        dma('sp', mlg[:], ml_norm_g.partition_broadcast(128), [], ['mlg'])
        CT = sb1("CT", [128, 4, 129]); mbc = sb1("mbc", [128, 4])
        fmq = [sb1("fmq%d" % i, [128, 8, 128]) for i in range(2)]
        mvo = [sb1("mvo%d" % i, [128, 1032]) for i in range(2)]
        ms = sb1("ms", [128, 96])
        MB = sb1("MB", [128, 8]); bcs = sb1("bcs", [128, 8])
        gT = sb1("gT", [4, 128])
        Gm = sb1("Gm", [128, 4, 128]); Dm = Gm; Sm = Gm
        STm = sb1("STm", [128, 4, 128]); svs = sb1("svs", [128, 512]); numt = sb1("numt", [128, 4, 128])
        sgm = svs; yml = yatt; vwx = sb1("vwx", [128, 4, 129])
        ktm = STm; junk = sb1("junk", [128, 128])

        def mlstm_tile(sq, t, ti, b2):
            r0 = ti * 128
            if t == 0:
                S.op('dve', lambda e: e.memset(CT[:], 0.0), [], ['CT'])
                S.op('dve', lambda e: e.memset(mbc[:], 0.0), [], ['mbc'])
            mqT = lambda h: fmq[b2][:, h, :]
            mkT = lambda h: fmq[b2][:, 4 + h, :]
            mv = lambda h: mvo[b2][:, h * 128:(h + 1) * 128]
            X = lambda a, b=None: ms[:, a:(a + 4 if b is None else b)]
            fq, mvt = 'fmq%d' % b2, 'mvo%d' % b2
            tt('dve', X(0, 8), mvo[b2][:, 1024:1032], gbias[:], ALU.add, [mvt, 'gbias'], ['m_gt'])
            stt(X(8), X(4), -1.0, X(4), ALU.mult, ALU.max, ['m_gt'], ['m_ax'])
            act(X(8), X(8), AF.Exp, ['m_ax'], ['m_ax'], scale=-1.0)
            act(X(8), X(8), AF.Ln, ['m_ax'], ['m_ax'], bias=1.0)
            ts('dve', X(12), X(4), 0.0, None, ALU.min, None, ['m_gt'], ['m_lf'])
            tt('dve', X(12), X(12), X(8), ALU.subtract, ['m_lf', 'm_ax'], ['m_lf'])
            if t == 0:
                ts('dve', X(0), X(0), padv[:, 0:1], padv[:, 1:2], ALU.mult, ALU.add, ['m_gt', 'padv'], ['m_gt'])
                ts('dve', X(12), X(12), padv[:, 0:1], None, ALU.mult, None, ['m_lf', 'padv'], ['m_lf'])
            yield
            bk = bank(0, 2)
            mm(pb[bk][:, 0:4], tri_incl[:], X(12), True, True, ['tri_incl', 'm_lf'], ['pb%d' % bk])
            cp('dve', MB[:, 4:8], pb[bk][:, 0:4], ['pb%d' % bk], ['MBb'])
            tt('dve', X(16), X(0), MB[:, 4:8], ALU.subtract, ['m_gt', 'MBb'], ['m_g'])
            yield
            bk = bank(0, 2)
            tr(pb[bk][0:4, 0:128], X(16), ['m_g'], ['pb%d' % bk])
            cp('act', gT[:, :], pb[bk][0:4, 0:128], ['pb%d' % bk], ['gT'])
            yield
            bg = sbank()
            for h in range(4):
                mm(pb[bg][:, h * 128:(h + 1) * 128], selh[0:4, h * 128:(h + 1) * 128], gT[0:4, :], True, True, ['selh', 'gT'], ['pb%d' % bg])
            for h in range(4):
                tt('dve', Gm[:, h, :], pb[bg][:, h * 128:(h + 1) * 128], cmask[:], ALU.add, ['pb%d' % bg, 'cmask'], ['Gm'])
            yield
            S.op('dve', lambda e: e.reduce_max(out=X(20), in_=Gm[:, :, :], axis=AX.X), ['Gm'], ['m_cm'])
            tt('dve', MB[:, 0:4], X(20), mbc[:], ALU.max, ['m_cm', 'mbc'], ['MBm'])
            if t > 0:
                ts('dve', X(24), MB[:, 0:4], -1.0, None, ALU.mult, None, ['MBm'], ['m_negM'])
                for h in range(4):
                    act(Dm[:, h, :], Gm[:, h, :], AF.Exp, ['Gm', 'm_negM'], ['Gm'], bias=ms[:, 24 + h:25 + h])
                yield
                bq = sbank()
                for h in range(4):
                    mm(pb[bq][:, h * 128:(h + 1) * 128], mqT(h), mkT(h), True, True, [fq], ['pb%d' % bq])
                tt('dve', Sm[:, :, :], pb[bq][:, :].rearrange("p (h r) -> p h r", h=4), Dm[:, :, :], ALU.mult, ['pb%d' % bq, 'Gm'], ['Gm'])
                S.op('dve', lambda e: e.reduce_sum(out=X(28), in_=Sm[:, :, :], axis=AX.X), ['Gm'], ['m_rs'])
                yield
                bt = sbank()
                for h in range(4):
                    tr(pb[bt][:, h * 128:(h + 1) * 128], Sm[:, h, :], ['Gm'], ['pb%d' % bt])
                cp('act', STm[:, :, :], pb[bt][:, :].rearrange("p (h r) -> p h r", h=4), ['pb%d' % bt], ['STm'])
                yield
                bo = sbank()
                for h in range(4):
                    mm(pb[bo][:, h * 128:(h + 1) * 128], STm[:, h, :], mv(h), True, True, ['STm', mvt], ['pb%d' % bo])
                cp('act', svs[:, :], pb[bo][:, :], ['pb%d' % bo], ['svs'])
                bu = [sbank(), sbank()]
                for h in range(4):
                    mm(pb[bu[h // 2]][:, (h % 2) * 129:(h % 2) * 129 + 129], mqT(h), CT[:, h, :], True, True, [fq, 'CT'], ['pb%d' % bu[h // 2]])
                tt('dve', X(32), mbc[:], MB[:, 0:4], ALU.subtract, ['mbc', 'MBm'], ['m_wi'])
                act(X(32), X(32), AF.Exp, ['m_wi'], ['m_wi'])
                for h in range(4):
                    stt(numt[:, h, :], pb[bu[h // 2]][:, (h % 2) * 129:(h % 2) * 129 + 128], ms[:, 32 + h:33 + h], svs[:, h * 128:(h + 1) * 128],
                        ALU.mult, ALU.add, ['pb%d' % bu[h // 2], 'm_wi', 'svs'], ['numt'])
                for hh in range(2):
                    cp('dve', ms[:, 36 + 2 * hh:38 + 2 * hh], pb[bu[hh]][:, 128:258:129], ['pb%d' % bu[hh]], ['m_uc%d' % hh])
                tt('dve', X(36), X(36), X(32), ALU.mult, ['m_uc0', 'm_uc1', 'm_wi'], ['m_den'])
                tt('dve', X(36), X(36), X(28), ALU.add, ['m_den', 'm_rs'], ['m_den'])
                stt(X(36), X(36), -1.0, X(36), ALU.mult, ALU.max, ['m_den'], ['m_den'])
                tt('dve', X(40), MB[:, 0:4], MB[:, 4:8], ALU.add, ['MBm', 'MBb'], ['m_em'])
                act(X(40), X(40), AF.Exp, ['m_em'], ['m_em'], scale=-1.0)
                tt('dve', X(36), X(36), X(40), ALU.max, ['m_den', 'm_em'], ['m_den'])
                S.op('dve', lambda e: e.reciprocal(out=X(44), in_=X(36)), ['m_den'], ['m_rden'])
                yield
                tt('dve', Gm[:, :, :], numt[:, :, :], numt[:, :, :], ALU.mult, ['numt', 'Gm'], ['Gm'])
                S.op('dve', lambda e: e.reduce_sum(out=X(48), in_=Gm[:, :, :], axis=AX.X), ['Gm'], ['m_ssq'])
                tt('dve', X(52), X(44), X(44), ALU.mult, ['m_rden'], ['m_q2'])
                tt('dve', X(52), X(52), X(48), ALU.mult, ['m_q2', 'm_ssq'], ['m_q2'])
                ts('dve', X(52), X(52), 1.0 / 128, EPS, ALU.mult, ALU.add, ['m_q2'], ['m_q2'])
                act(X(52), X(52), AF.Ln, ['m_q2'], ['m_q2'])
                act(X(52), X(52), AF.Exp, ['m_q2'], ['m_q2'], scale=-0.5)
                tt('dve', X(56), X(52), X(44), ALU.mult, ['m_q2', 'm_rden'], ['m_scl'])
                yield
                act(sgm[:, :], mvo[b2][:, 512:1024], AF.Exp, [mvt], ['svs'], scale=-1.0)
                ts('dve', sgm[:, :], sgm[:, :], 1.0, None, ALU.add, None, ['svs'], ['svs'])
                S.op('dve', lambda e: e.reciprocal(out=sgm[:, :], in_=sgm[:, :]), ['svs'], ['svs'])
                tt('pool', sgm[:, :], sgm[:, :], mlg[:], ALU.mult, ['svs', 'mlg'], ['svs'])
                for h in range(4):
                    stt(yml[:, h * 128:(h + 1) * 128], numt[:, h, :], ms[:, 56 + h:57 + h], sgm[:, h * 128:(h + 1) * 128],
                        ALU.mult, ALU.mult, ['numt', 'm_scl', 'svs'], ['yatt'])
                yield
                bk = bank(0, 2)
                for h in range(4):
                    tr(pb[bk][:, h * 128:(h + 1) * 128], yml[:, h * 128:(h + 1) * 128], ['yatt'], ['pb%d' % bk])
                cp('act', ytile[b2][:, 4:8, :], pb[bk][:, :].rearrange("p (h t) -> p h t", h=4), ['pb%d' % bk], ['ytile%d' % b2])
                dma('sp', YT[ti, :, 4:8, :], ytile[b2][:, 4:8, :], ['ytile%d' % b2], ['YTm_%d' % ti])
            if t == NT - 1:
                return
            yield
            yield
            bk = bank(0, 2)
            mm(pb[bk][:, 0:8], sel127[:], MB[:, :], True, True, ['sel127', 'MBm', 'MBb'], ['pb%d' % bk])
            cp('dve', bcs[:, :], pb[bk][:, 0:8], ['pb%d' % bk], ['bcs'])
            tt('dve', X(60), X(16), bcs[:, 0:4], ALU.subtract, ['m_g', 'bcs'], ['m_wr'])
            act(X(60), X(60), AF.Exp, ['m_wr'], ['m_wr'])
            tt('dve', X(64), mbc[:], bcs[:, 0:4], ALU.subtract, ['mbc', 'bcs'], ['m_wo'])
            act(X(64), X(64), AF.Exp, ['m_wo'], ['m_wo'])
            tt('dve', mbc[:], bcs[:, 4:8], bcs[:, 0:4], ALU.add, ['bcs'], ['mbc'])
            yield
            for h in range(4):
                ts('pool', vwx[:, h, 0:128], mv(h), ms[:, 60 + h:61 + h], 1.0, ALU.mult, ALU.mult, [mvt, 'm_wr'], ['vwx'])
            cp('pool', vwx[:, :, 128:129], X(60).rearrange("p (h o) -> p h o", o=1), ['m_wr'], ['vwx'])
            bt = sbank()
            for h in range(4):
                tr(pb[bt][:, h * 128:(h + 1) * 128], mkT(h), [fq], ['pb%d' % bt])
            cp('act', ktm[:, :, :], pb[bt][:, :].rearrange("p (h r) -> p h r", h=4), ['pb%d' % bt], ['STm'])
            yield
            bc2 = [sbank(), sbank()]
            for h in range(4):
                mm(pb[bc2[h // 2]][:, (h % 2) * 129:(h % 2) * 129 + 129], ktm[:, h, :], vwx[:, h, :], True, True, ['STm', 'vwx'], ['pb%d' % bc2[h // 2]])
            for h in range(4):
                stt(CT[:, h, :], CT[:, h, :], ms[:, 64 + h:65 + h], pb[bc2[h // 2]][:, (h % 2) * 129:(h % 2) * 129 + 129],
                    ALU.mult, ALU.add, ['CT', 'm_wo', 'pb%d' % bc2[h // 2]], ['CT'])

        def kchunks(nk):
            bounds = [0]
            nxt = 400
            while nxt < nk:
                bounds.append(nxt)
                nxt += 512
            bounds.append(nk)
            return [(bounds[i], bounds[i + 1]) for i in range(len(bounds) - 1)]

        sbk = [0]
        stc = [0]; ptc = [0]
        zt = sb1("zt", [128, 8192], BF16)
        S.op('pool', lambda e: e.memset(zt[:], 0.0), [], ['zt'])
        XSv = XS[0:E * C, :].rearrange("(p r) d -> p (r d)", p=128)
        per_p = (E * C // 128) * D
        zf_chunks = [(c0_, min(8192, per_p - c0_)) for c0_ in range(0, per_p, 8192)]
        zf_per_tile = -(-len(zf_chunks) // max(1, NTILE - 1))
        zf_i = [0]

        def zero_fill_some():
            for _ in range(zf_per_tile):
                if zf_i[0] < len(zf_chunks):
                    c0_, n_ = zf_chunks[zf_i[0]]
                    zf_i[0] += 1
                    dma('sp', XSv[:, c0_:c0_ + n_], zt[:, 0:n_], ['zt'], ['XS0_%d' % c0_])

        def head_done(h):
            rg = (h % 2) * 129
            hs = slice(h * 128, (h + 1) * 128)
            S.op('dve', lambda e: e.reciprocal(out=att_s[:, 2 * h:2 * h + 1], in_=pb[6][:, rg + 128:rg + 129]), ['pb6'], ['att_ra%d' % h])
            S.op('dve', lambda e: e.reciprocal(out=att_s[:, 2 * h + 1:2 * h + 2], in_=pb[7][:, rg + 128:rg + 129]), ['pb7'], ['att_rb%d' % h])
            tt('dve', att_s[:, 8 + h:9 + h], att_s[:, 2 * h + 1:2 * h + 2], lams[:, 5:6], ALU.mult, ['att_rb%d' % h, 'lams'], ['att_c%d' % h])
            ts('dve', otmp[:, hs], pb[7][:, rg:rg + 128], att_s[:, 8 + h:9 + h], None, ALU.mult, None, ['pb7', 'att_c%d' % h], ['otmp'])
            stt(osb[:, hs], pb[6][:, rg:rg + 128], att_s[:, 2 * h:2 * h + 1], otmp[:, hs], ALU.mult, ALU.add, ['pb6', 'att_ra%d' % h, 'otmp'], ['osb'])

        def sbank():
            sbk[0] += 1
            return 2 + sbk[0] % 2

        def issue_loads(tj):
            tq = tj % NT
            bq = tj % 2
            rq = tj * 128
            zq = ['ZQ%d_%d' % (p_, tj) for p_ in range(3)]
            dma('sp', qkin[bq][:], ZQKV[rq:rq + 128, 0:1024], zq, ['qkin%d' % bq])
            dma('sp', fmq[bq][:], ZFM[tj, :, 0:8, :], ['ZFM_%d' % tj], ['fmq%d' % bq])
            dma('sp', mvo[bq][:], ZTM[rq:rq + 128, 0:1032], ['ZTM_%d' % tj], ['mvo%d' % bq])
            if tq == 0:
                dma('sp', Vc[0:16, 0, :, 0:128], ZQKV[rq + 112:rq + 128, 1024:1536].rearrange("p (h e) -> p h e", h=4),
                    zq + ['Vones'], ['Vc%d' % tq])
            else:
                dma('sp', Vc[:, tq, :, 0:128], ZQKV[rq:rq + 128, 1024:1536].rearrange("p (h e) -> p h e", h=4),
                    zq + ['Vones'], ['Vc%d' % tq])

        for sq in range(NS):
            for t in range(NT):
                ti = sq * NT + t
                b2 = ti % 2
                r0 = ti * 128
                if ti == 0:
                    issue_loads(0)
                if ti + 1 < NTILE:
                    issue_loads(ti + 1)
                mgen = mlstm_tile(sq, t, ti, b2)
                next(mgen, None)
                if t == 0:
                    kc0, kn = 0, 16
                else:
                    kc0, kn = 16 + (t - 1) * 128, 128
                bk = bank(0, 2)
                pvk = pb[bk][:, :].bitcast(BF16)[:, 0:512]
                for h in range(4):
                    S.op('pe', lambda e, h=h, pvk=pvk, b2=b2: e.transpose(pvk[:, h * 128:(h + 1) * 128], qkin[b2][:, 512 + h * 128:512 + (h + 1) * 128], identb[:]),
                         ['qkin%d' % b2, 'identb'], ['pb%d' % bk])
                cp('act', KT[:, :, kc0:kc0 + kn], pvk.rearrange("p (h t) -> p h t", h=4)[:, :, 128 - kn:128],
                   ['pb%d' % bk], ['KT'])
                tt('dve', sqt[:], qkin[b2][:, 512:1024], qkin[b2][:, 512:1024], ALU.mult, ['qkin%d' % b2], ['sqt'])
                S.op('dve', lambda e: e.reduce_sum(out=nrm[:, 0:8], in_=sqt[:, :].rearrange("p (g d) -> p g d", g=8), axis=AX.X), ['sqt'], ['nrm_k'])
                bk = bank(0, 2)
                tr(pb[bk][0:8, 0:128], nrm[:, 0:8], ['nrm_k'], ['pb%d' % bk])
                if t == 0:
                    S.op('dve', lambda e, bk=bk: e.reduce_max(out=kmx[:, 0:1], in_=pb[bk][0:8, 0:128], axis=AX.X), ['pb%d' % bk], ['kmx'])
                else:
                    S.op('dve', lambda e, bk=bk: e.reduce_max(out=kmx[:, 1:2], in_=pb[bk][0:8, 0:128], axis=AX.X), ['pb%d' % bk], ['kmx1'])
                    tt('dve', kmx[:, 0:1], kmx[:, 0:1], kmx[:, 1:2], ALU.max, ['kmx', 'kmx1'], ['kmx'])
                if t == 0:
                    for _ in mgen:
                        pass
                    continue
                bk = bank(0, 2)
                pvq = pb[bk][:, :].bitcast(BF16)[:, 0:512]
                for h in range(4):
                    S.op('pe', lambda e, h=h, pvq=pvq, b2=b2: e.transpose(pvq[:, h * 128:(h + 1) * 128], qkin[b2][:, h * 128:(h + 1) * 128], identb[:]),
                         ['qkin%d' % b2, 'identb'], ['pb%d' % bk])
                cp('act', qT[:, :, :], pvq.rearrange("p (h t) -> p h t", h=4), ['pb%d' % bk], ['qT'])
                tt('dve', sqt[:], qkin[b2][:, 0:512], qkin[b2][:, 0:512], ALU.mult, ['qkin%d' % b2], ['sqt'])
                S.op('dve', lambda e: e.reduce_sum(out=nrm[:, 8:16], in_=sqt[:, :].rearrange("p (g d) -> p g d", g=8), axis=AX.X), ['sqt'], ['nrm_q'])
                bk = bank(0, 2)
                tr(pb[bk][0:8, 0:128], nrm[:, 8:16], ['nrm_q'], ['pb%d' % bk])
                S.op('dve', lambda e, bk=bk: e.reduce_max(out=kmx[:, 2:3], in_=pb[bk][0:8, 0:128], axis=AX.X), ['pb%d' % bk], ['qmx'])
                tt('dve', kmx[:, 3:4], kmx[:, 2:3], kmx[:, 0:1], ALU.mult, ['qmx', 'kmx'], ['cprod'])
                act(kmx[:, 3:4], kmx[:, 3:4], AF.Ln, ['cprod'], ['cprod'])
                act(kmx[:, 3:4], kmx[:, 3:4], AF.Exp, ['cprod'], ['cprod'], scale=0.5)
                ts('dve', kmx[:, 4:5], kmx[:, 3:4], -0.125, None, ALU.mult, None, ['cprod'], ['cneg'])
                ts('dve', dg8[:, :], ident[0:8, 0:8], kmx[:, 4:5], None, ALU.mult, None, ['cneg', 'ident'], ['dg8'])
                bk = bank(0, 2)
                mm(pb[bk][:, 0:8], onesb1[0:8, :], dg8[0:8, 0:8], True, True, ['onesb1', 'dg8'], ['pb%d' % bk])
                cp('dve', negc[:, :], pb[bk][:, 0:8], ['pb%d' % bk], ['negc'])
                blocks = [(0, 16, 0)] + [(16 + (j - 1) * 128, 128, j) for j in range(1, t + 1)]
                groups = [blocks[g0:g0 + 4] for g0 in range(0, len(blocks), 4)]

                def st_exp(h, grp):
                    pr = stc[0] % 2
                    stc[0] += 1
                    res = []
                    bks = (2 + 2 * pr, 3 + 2 * pr)
                    for i, (c0, n, j) in enumerate(grp):
                        for m in range(2):
                            ps_ = slice(m * 64, (m + 1) * 64)
                            mm(pb[bks[m]][0:n, i * 128:(i + 1) * 128], KT[ps_, h, c0:c0 + n], qT[ps_, h, :], True, True, ['KT', 'qT'], ['pb%d' % bks[m]])
                    for m in range(2):
                        hm = h * 2 + m
                        bk = bks[m]
                        slot = ptc[0] % 6
                        ptc[0] += 1
                        ptok = 'PTb%d' % slot
                        if grp[-1][2] == t:
                            i = len(grp) - 1
                            tt('dve', pb[bk][:, i * 128:(i + 1) * 128], pb[bk][:, i * 128:(i + 1) * 128], cmaskT[:], ALU.add,
                               ['pb%d' % bk, 'cmaskT'], ['pb%d' % bk])
                        lo = 0
                        if grp[0][1] == 16:
                            act(PTb[slot][0:16, 0:128], pb[bk][0:16, 0:128], AF.Exp, ['pb%d' % bk, 'negc'], [ptok], bias=negc[0:16, hm:hm + 1], scale=0.125)
                            lo = 128
                        hi = 128 * len(grp)
                        if hi > lo:
                            act(PTb[slot][:, lo:hi], pb[bk][:, lo:hi], AF.Exp, ['pb%d' % bk, 'negc'], [ptok], bias=negc[:, hm:hm + 1], scale=0.125)
                        res.append((slot, ptok))
                    return res

                def pv(h, grp, res):
                    rg = (h % 2) * 129
                    for m in range(2):
                        slot, ptok = res[m]
                        for i, (c0, n, j) in enumerate(grp):
                            mm(pb[6 + m][:, rg:rg + 129], PTb[slot][0:n, i * 128:(i + 1) * 128], Vc[0:n, j, h, :],
                               j == 0, j == t, [ptok, 'Vc%d' % j], ['pb%d' % (6 + m)])

                work = [(h, grp) for h in range(4) for grp in groups]
                prev = None
                for wi_, (h, grp) in enumerate(work):
                    res = st_exp(h, grp)
                    if prev is not None:
                        pv(*prev)
                        if prev[0] != h:
                            head_done(prev[0])
                    prev = (h, grp, res)
                    if wi_ == 1:
                        zero_fill_some()
                    if wi_ % 2 == 1:
                        next(mgen, None)
                pv(*prev)
                head_done(3)
                for _ in mgen:
                    pass
                tt('dve', otmp[:, :], osb[:, :], osb[:, :], ALU.mult, ['osb', 'otmp'], ['otmp'])
                S.op('dve', lambda e: e.reduce_sum(out=att_s[:, 40:44], in_=otmp[:, :].rearrange("p (h e) -> p h e", h=4), axis=AX.X), ['otmp'], ['att_ss'])
                ts('dve', att_s[:, 40:44], att_s[:, 40:44], 1.0 / 128, EPS, ALU.mult, ALU.add, ['att_ss'], ['att_ss'])
                act(att_s[:, 40:44], att_s[:, 40:44], AF.Ln, ['att_ss'], ['att_ss'])
                act(att_s[:, 40:44], att_s[:, 40:44], AF.Exp, ['att_ss'], ['att_ss'], scale=-0.5)
                for h in range(4):
                    stt(yatt[:, h * 128:(h + 1) * 128], osb[:, h * 128:(h + 1) * 128], att_s[:, 40 + h:41 + h], gatt[:],
                        ALU.mult, ALU.mult, ['osb', 'att_ss', 'gatt'], ['yatt'])
                bk = bank(0, 2)
                for h in range(4):
                    tr(pb[bk][:, h * 128:(h + 1) * 128], yatt[:, h * 128:(h + 1) * 128], ['yatt'], ['pb%d' % bk])
                cp('act', ytile[b2][:, 0:4, :], pb[bk][:, :].rearrange("p (h t) -> p h t", h=4), ['pb%d' % bk], ['ytile%d' % b2])
                dma('sp', YT[ti, :, 0:4, :], ytile[b2][:, 0:4, :], ['ytile%d' % b2], ['YTa_%d' % ti])
        S.finish()
        S.barrier()
        p1.close()


        p2 = ExitStack()

        def sb2(name, shape, dt=F32):
            return p2.enter_context(nc.sbuf_tensor(name, list(shape), dt))

        QG = min(4, NT - 1)
        QW = QG * 128
        assert (NT - 1) % QG == 0
        load_ln(sb2, 2)
        wrt = sb2("wrt", [128, 8, E]); brt = sb2("brt", [1, E]); ones = sb2("ones", [128, 128])
        dma('sp', wrt[:], w_router.rearrange("(k p) e -> p k e", p=128), [], ['wrt'])
        dma('sp', brt[:], b_router[:, :], [], ['brt'])
        dma('sp', ones[:], cst['ones'][:, :], [], ['ones'])
        tri_excl = sb2("tri_excl", [128, 128]); eoff = sb2("eoff_s", [128, E]); ie1 = sb2("ie1_s", [128, E])
        dma('sp', tri_excl[:], cst['tri_excl'][:, :], [], ['tri_excl'])
        dma('sp', eoff[:], eoff_d[:, :], [], ['eoff'])
        dma('sp', ie1[:], ie1_d[:, :], [], ['ie1'])
        basecnt = sb2("basecnt", [128, E])
        S.op('dve', lambda e: e.memset(basecnt[:], 0.0), [], ['basecnt'])
        yin = sb2("yin0", [128, 8, QW], BF16)
        gin = sb2("gin0", [128, 16, QW], BF16)
        mrgT = sb2("mrgT", [128, 8, QW], BF16)
        t12 = [sb2("t12_%d" % i, [128, QW]) for i in range(4)]
        NJ = QG
        h0in = [sb2("h0in%d" % i, [128, D]) for i in range(NJ)]
        r1 = [sb2("r1_%d" % i, [128, D]) for i in range(NJ)]
        lntmp2 = [sb2("lntmp2_%d" % i, [128, D]) for i in range(NJ)]
        lnst2 = [sb2("lnst2_%d" % i, [128, 16]) for i in range(NJ)]
        h1 = [sb2("h1_%d" % i, [128, D]) for i in range(NJ)]
        h1T = [sb2("h1T%d" % i, [128, 8, 128]) for i in range(NJ)]
        h1b = [sb2("h1b%d" % i, [128, D], BF16) for i in range(NJ)]
        rsl = [sb2("rs_%d" % i, [128, 64]) for i in range(NJ)]
        lgl = [sb2("lg%d" % i, [128, E]) for i in range(NJ)]; mskl = [sb2("msk%d" % i, [128, E]) for i in range(NJ)]
        exgl = [sb2("exg%d" % i, [128, E]) for i in range(NJ)]; posl = [sb2("posf%d" % i, [128, E]) for i in range(NJ)]
        ohel = [sb2("ohe%d" % i, [128, E]) for i in range(NJ)]
        xs_tokens = []
        xs0_tokens = [k for k in S.last_w if k.startswith('XS0_')]

        def b2_tile(sq, gq, j):
            q = j
            ti = sq * NT + 1 + gq * QG + j
            xi = sq * (NT - 1) + gq * QG + j
            T = lambda nm: '%s_%d' % (nm, q)
            rs_, lg, msk, exg, posf, ohe = rsl[q], lgl[q], mskl[q], exgl[q], posl[q], ohel[q]
            dma('sp', h0in[q][:], H0[ti * 128:(ti + 1) * 128, :], ['H0_%d' % ti], [T('h0in')])
            yield
            for half in range(2):
                bk = bank(2, 8)
                for kc in range(8):
                    mm(pb[bk][:, :], mrgT[:, kc, j * 128:(j + 1) * 128], wo[:, kc, half * 512:(half + 1) * 512], kc == 0, kc == 7,
                       ['mrgT', 'wo'], ['pb%d' % bk])
                stt(r1[q][:, half * 512:(half + 1) * 512], h0in[q][:, half * 512:(half + 1) * 512], ALPHA, pb[bk][:, :],
                    ALU.mult, ALU.add, [T('h0in'), 'pb%d' % bk], [T('r1')])
            yield
            x_ap, stat, tmp = r1[q][:], lnst2[q], lntmp2[q][:]
            for c2 in range(2):
                S.op('dve', lambda e, c2=c2: e.bn_stats(out=stat[:, c2 * 6:(c2 + 1) * 6], in_=x_ap[:, c2 * 512:(c2 + 1) * 512]),
                     [T('r1')], [T('st%d' % c2)])
            yield
            S.op('dve', lambda e: e.bn_aggr(out=stat[:, 12:14], in_=stat[:, 0:12].rearrange("p (c s) -> p c s", s=6)),
                 [T('st0'), T('st1')], [T('ag')])
            yield
            act(stat[:, 14:15], stat[:, 13:14], AF.Sqrt, [T('ag')], [T('sd')], bias=epsb[:, 0:1])
            yield
            S.op('dve', lambda e: e.reciprocal(out=stat[:, 15:16], in_=stat[:, 14:15]), [T('sd')], [T('rsd')])
            stt(tmp, x_ap, stat[:, 12:13], lnbh[0][:, 0, :], ALU.subtract, ALU.mult, [T('r1'), T('ag'), 'lnb2'], [T('lntmp')])
            yield
            stt(h1[q][:], tmp, stat[:, 15:16], lnbh[0][:, 1, :], ALU.mult, ALU.add, [T('lntmp'), T('rsd'), 'lnb3'], [T('h1')])
            dma('sp', H1[xi * 128:(xi + 1) * 128, :], h1[q][:], [T('h1')], ['H1_%d' % xi])
            cp('act', h1b[q][:], h1[q][:], [T('h1')], [T('h1b')])
            yield
            for kq in range(2):
                bk = bank(0, 2)
                for k4 in range(4):
                    k = kq * 4 + k4
                    tr(pb[bk][:, k4 * 128:(k4 + 1) * 128], h1[q][:, k * 128:(k + 1) * 128], [T('h1')], ['pb%d' % bk])
                cp('act' if kq == 0 else 'dve', h1T[q][:, kq * 4:(kq + 1) * 4, :], pb[bk][:, :].rearrange("p (k t) -> p k t", k=4),
                   ['pb%d' % bk], [T('h1T')])
            yield
            bk = bank(0, 2)
            for k in range(8):
                mm(pb[bk][:, 0:E], h1T[q][:, k, :], wrt[:, k, :], k == 0, False, [T('h1T'), 'wrt'], ['pb%d' % bk])
            mm(pb[bk][:, 0:E], ones[0:1, :], brt[0:1, :], False, True, ['ones', 'brt'], ['pb%d' % bk])
            cp('dve', lg[:], pb[bk][:, 0:E], ['pb%d' % bk], [T('lg')])
            yield
            S.op('dve', lambda e: e.max(out=rs_[:, 0:8], in_=lg[:]), [T('lg')], [T('r_mx')])
            yield
            ts('dve', msk[:], lg[:], rs_[:, 3:4], None, ALU.is_ge, None, [T('lg'), T('r_mx')], [T('msk')])
            ts('dve', rs_[:, 8:9], rs_[:, 0:1], -1.0, None, ALU.mult, None, [T('r_mx')], [T('r_nm')])
            yield
            act(exg[:], lg[:], AF.Exp, [T('lg'), T('r_nm')], [T('exg')], bias=rs_[:, 8:9])
            bk = bank(0, 2)
            mm(pb[bk][:, 0:E], tri_excl[:], msk[:], True, True, ['tri_excl', T('msk')], ['pb%d' % bk])
            tt('dve', posf[:], pb[bk][:, 0:E], basecnt[:], ALU.add, ['pb%d' % bk, 'basecnt'], [T('posf')])
            bk = bank(0, 2)
            mm(pb[bk][:, 0:E], ones[:], msk[:], True, True, ['ones', T('msk')], ['pb%d' % bk])
            tt('dve', basecnt[:], basecnt[:], pb[bk][:, 0:E], ALU.add, ['pb%d' % bk, 'basecnt'], ['basecnt'])
            yield
            tt('dve', exg[:], exg[:], msk[:], ALU.mult, [T('exg'), T('msk')], [T('exg')])
            tt('dve', posf[:], posf[:], eoff[:], ALU.add, [T('posf'), 'eoff'], [T('posf')])
            yield
            S.op('dve', lambda e: e.reduce_sum(out=rs_[:, 9:10], in_=exg[:], axis=AX.X), [T('exg')], [T('r_sum')])
            tt('dve', posf[:], posf[:], msk[:], ALU.mult, [T('posf'), T('msk')], [T('posf')])
            yield
            S.op('dve', lambda e: e.reciprocal(out=rs_[:, 10:11], in_=rs_[:, 9:10]), [T('r_sum')], [T('r_rs')])
            S.op('dve', lambda e: e.max(out=rs_[:, 16:24], in_=posf[:]), [T('posf')], [T('r_v8')])
            yield
            ts('dve', exg[:], exg[:], rs_[:, 10:11], None, ALU.mult, None, [T('exg'), T('r_rs')], [T('exg')])
            ts('dve', slots_all[:, xi, :], rs_[:, 16:20], -1.0, None, ALU.add, None, [T('r_v8')], ['slots%d' % xi])
            tt('dve', ohe[:], msk[:], ie1[:], ALU.mult, [T('msk'), 'ie1'], [T('ohe')])
            yield
            for k in range(4):
                tok = 'XS_%d_%d' % (xi, k)
                xs_tokens.append(tok)
                S.dma('pool', lambda e, xi=xi, k=k, q=q: e.indirect_dma_start(
                    out=XS[0:E * C, :], out_offset=bass.IndirectOffsetOnAxis(ap=slots_all[:, xi, k:k + 1], axis=0),
                    in_=h1b[q][:], in_offset=None), [T('h1b'), 'slots%d' % xi] + xs0_tokens, [tok])
            S.op('dve', lambda e: e.max(out=rs_[:, 24:32], in_=ohe[:]), [T('ohe')], [T('r_e8')])
            yield
            for k in range(4):
                ts('dve', ohe[:], ie1[:], rs_[:, 24 + k:25 + k], None, ALU.is_equal, None, ['ie1', T('r_e8')], [T('ohe')])
                yield
                tt('dve', ohe[:], ohe[:], exg[:], ALU.mult, [T('ohe'), T('exg')], [T('ohe')])
                yield
                S.op('dve', lambda e, xi=xi, k=k: e.reduce_sum(out=gates_all[:, xi, k:k + 1], in_=ohe[:], axis=AX.X), [T('ohe')], ['gates%d' % xi])
                yield

        prev_gens = []
        for sq in range(NS):
            for gq in range((NT - 1) // QG):
                ti0 = sq * NT + 1 + gq * QG
                for j in range(QG):
                    dma('sp', yin[:, :, j * 128:(j + 1) * 128], YT[ti0 + j, :, :, :],
                        ['YTa_%d' % (ti0 + j), 'YTm_%d' % (ti0 + j)], ['yin0'])
                    dma('sp', gin[:, :, j * 128:(j + 1) * 128], ZG[ti0 + j, :, :, :], ['ZG_%d' % (ti0 + j)], ['gin0'])
                for dc in range(8):
                    ba = bank(2, 8)
                    for kc in range(4):
                        mm(pb[ba][:, 0:QW], wa[:, kc, dc * 128:(dc + 1) * 128], yin[:, kc, :], kc == 0, kc == 3, ['wa', 'yin0'], ['pb%d' % ba])
                    bm = bank(2, 8)
                    for kc in range(4):
                        mm(pb[bm][:, 0:QW], wm_[:, kc, dc * 128:(dc + 1) * 128], yin[:, 4 + kc, :], kc == 0, kc == 3, ['wm', 'yin0'], ['pb%d' % bm])
                    tb = 2 * (dc % 2)
                    tt('dve', t12[tb][:], pb[ba][:, 0:QW], gin[:, dc, :], ALU.mult, ['pb%d' % ba, 'gin0'], ['t12_%d' % tb])
                    tt('dve', t12[tb + 1][:], pb[bm][:, 0:QW], gin[:, 8 + dc, :], ALU.mult, ['pb%d' % bm, 'gin0'], ['t12_%d' % (tb + 1)])
                    tt('pool', mrgT[:, dc, :], t12[tb][:], t12[tb + 1][:], ALU.add, ['t12_%d' % tb, 't12_%d' % (tb + 1)], ['mrgT'])
                gens = [b2_tile(sq, gq, j) for j in range(QG)]
                live = list(gens)
                while live:
                    for gnr in list(live):
                        try:
                            next(gnr)
                        except StopIteration:
                            live.remove(gnr)
        S.finish()
        S.barrier()
        p2.close()
        pw.close()

        p3 = ExitStack()

        def sb3(name, shape, dt=F32):
            return p3.enter_context(nc.sbuf_tensor(name, list(shape), dt))

        NST = C // 128
        NWB = 8
        wbuf = [sb3("wbuf%d" % i, [128, 8 * 512], BF16) for i in range(NWB)]
        XTL = [sb3("XT%d" % i, [128, 8, C], BF16) for i in range(2)]; actT = sb3("actT", [128, 8, C], BF16)
        xsin = [sb3("xsin%d" % i, [128, D], BF16) for i in range(NST)]
        identc = sb3("identc", [128, 128], BF16)
        cp('dve', identc[:], ident[:], ['ident'], ['identc'])
        ystg = [sb3("ystg%d" % i, [128, D], BF16) for i in range(2)]
        bdb = [sb3("bdb%d" % i, [128, D]) for i in range(2)]
        gtmp = [sb3("gtmp%d" % i, [128, 512]) for i in range(2)]; stmp = [sb3("stmp%d" % i, [128, 512]) for i in range(2)]
        ltmp = [sb3("ltmp%d" % i, [128, 512]) for i in range(2)]
        ci_ = [0]
        bgs = [sb3("bgs%d" % i, [128, 16]) for i in range(2)]
        ones1 = sb3("ones1", [1, 128])
        dma('sp', ones1[:], cst['ones'][0:1, :], [], ['ones1'])
        wi = [0]
        xsc = [0]
        tgs = [(t0, min(512, C - t0)) for t0 in range(0, C, 512)]

        def need(gidx):
            while wi[0] <= min(gidx + 5, E * 6 - 1):
                gp = wi[0]
                wi[0] += 1
                ex_, pi_ = gp // 6, gp % 6
                wb_ = gp % NWB
                if pi_ < 4:
                    dma('pool', wbuf[wb_][:, :].rearrange("p (k c) -> p k c", k=8),
                        w_gu[ex_, :, pi_ * 512:(pi_ + 1) * 512].rearrange("(k p) c -> p k c", p=128), [], ['wbuf%d' % wb_])
                else:
                    dma('pool', wbuf[wb_][:, :].rearrange("p (k c) -> p k c", k=4),
                        w_down[ex_, (pi_ - 4) * 512:(pi_ - 3) * 512, :].rearrange("(k p) c -> p k c", p=128), [], ['wbuf%d' % wb_])
        def prep_gen(ex):
            XTn, xk = XTL[ex % 2], 'XT%d' % (ex % 2)
            for stt_ in range(NST):
                xb = stt_
                bk = bank(0, 2)
                pvb = pb[bk][:, :].bitcast(BF16)
                for k in range(8):
                    S.op('pe', lambda e, k=k, xb=xb, pvb=pvb: e.transpose(pvb[:, k * 128:(k + 1) * 128], xsin[xb][:, k * 128:(k + 1) * 128], identc[:]),
                         ['xsin%d' % xb, 'identc'], ['pb%d' % bk])
                cp('act' if stt_ % 2 == 0 else 'dve', XTn[:, :, stt_ * 128:(stt_ + 1) * 128],
                   pvb.rearrange("p (k t) -> p k t", k=8), ['pb%d' % bk], [xk])
                yield

        def issue_xs(ex_):
            for stt_ in range(NST):
                r0_ = ex_ * C + stt_ * 128
                dma('sp', xsin[stt_][:], XS[r0_:r0_ + 128, :], xs_tokens, ['xsin%d' % stt_])

        def load_bias(ex_):
            eb_ = ex_ % 2
            dma('sp', bgs[eb_][:], bguh[ex_, :, :], [], ['bgs%d' % eb_])
            ts('dve', bgs[eb_][:, 8:16], bgs[eb_][:, 8:16], 1.0, None, ALU.add, None, ['bgs%d' % eb_], ['bgs%d' % eb_])
            dma('sp', bdb[eb_][:], b_down[ex_:ex_ + 1, :].partition_broadcast(128), [], ['bdb%d' % eb_])

        issue_xs(0)
        for _ in prep_gen(0):
            pass
        for ex in range(E):
            eb = ex % 2
            XTc, xtk = XTL[ex % 2], 'XT%d' % (ex % 2)
            if ex == 0:
                load_bias(0)
            if ex + 1 < E:
                load_bias(ex + 1)
                issue_xs(ex + 1)
            wl = [(ex * 6 + pi) % NWB for pi in range(6)]
            need(ex * 6)
            for fc in range(8):
                need(ex * 6 + fc // 2)
                wb = wl[fc // 2]
                wv = wbuf[wb][:, :].rearrange("p (k c) -> p k c", k=8)
                base = (fc % 2) * 256
                for (t0, n) in tgs:
                    bg_ = bank(2, 8)
                    for k in range(8):
                        mm(pb[bg_][:, 0:n], wv[:, k, base:base + 256:2], XTc[:, k, t0:t0 + n], k == 0, k == 7, ['wbuf%d' % wb, xtk], ['pb%d' % bg_])
                    bl_ = bank(2, 8)
                    for k in range(8):
                        mm(pb[bl_][:, 0:n], wv[:, k, base + 1:base + 256:2], XTc[:, k, t0:t0 + n], k == 0, k == 7, ['wbuf%d' % wb, xtk], ['pb%d' % bl_])
                    q_ = ci_[0] % 2
                    ci_[0] += 1
                    G_, S_, L_ = gtmp[q_], stmp[q_], ltmp[q_]
                    gk, sk, lk = 'gtmp%d' % q_, 'stmp%d' % q_, 'ltmp%d' % q_
                    ts('dve', G_[:, 0:n], pb[bg_][:, 0:n], bgs[eb][:, fc:fc + 1], 7.0, ALU.add, ALU.min, ['pb%d' % bg_, 'bgs%d' % eb], [gk])
                    act(S_[:, 0:n], G_[:, 0:n], AF.Sigmoid, [gk], [sk], scale=1.702)
                    ts('dve', L_[:, 0:n], pb[bl_][:, 0:n], bgs[eb][:, 8 + fc:9 + fc], 8.0, ALU.add, ALU.min, ['pb%d' % bl_, 'bgs%d' % eb], [lk])
                    tt('dve', G_[:, 0:n], G_[:, 0:n], S_[:, 0:n], ALU.mult, [gk, sk], [gk])
                    stt(actT[:, fc, t0:t0 + n], L_[:, 0:n], -6.0, G_[:, 0:n], ALU.max, ALU.mult, [lk, gk], ['actT'])
            need(ex * 6 + 5)
            pg = prep_gen(ex + 1) if ex + 1 < E else iter(())
            for stt_ in range(NST):
                yb = stt_ % 2
                for half in range(2):
                    bk = bank(2, 8)
                    for fc in range(8):
                        wb = wl[4 + fc // 4]
                        wv = wbuf[wb][:, :].rearrange("p (k c) -> p k c", k=4)
                        mm(pb[bk][:, :], actT[:, fc, stt_ * 128:(stt_ + 1) * 128], wv[:, fc % 4, half * 512:(half + 1) * 512], fc == 0, fc == 7,
                           ['actT', 'wbuf%d' % wb], ['pb%d' % bk])
                    tt('dve', ystg[yb][:, half * 512:(half + 1) * 512], pb[bk][:, :], bdb[eb][:, half * 512:(half + 1) * 512], ALU.add,
                       ['pb%d' % bk, 'bdb%d' % eb], ['ystg%d' % yb])
                r0 = ex * C + stt_ * 128
                dma('sp', YS[r0:r0 + 128, :], ystg[yb][:], ['ystg%d' % yb], ['YS_%d_%d' % (ex, stt_)])
                next(pg, None)
            for _ in pg:
                pass
        S.finish()
        S.barrier()
        p3.close()

        p4 = ExitStack()

        def sb4(name, shape, dt=F32):
            return p4.enter_context(nc.sbuf_tensor(name, list(shape), dt))

        load_ln(sb4, 4)
        ys_tokens = ['YS_%d_%d' % (ex, q) for ex in range(E) for q in range(NST)]
        ND = 4
        yk = [[sb4("yk%d_%d" % (i, k), [128, D], BF16) for k in range(4)] for i in range(ND)]
        h1in = [sb4("h1in%d" % i, [128, D]) for i in range(ND)]
        accd = [sb4("accd%d" % i, [128, D]) for i in range(2)]
        lntmp4 = [sb4("lntmp4_%d" % i, [128, D]) for i in range(2)]; lnst4 = [sb4("lnst4_%d" % i, [128, 16]) for i in range(2)]
        od = [sb4("od%d" % i, [128, D]) for i in range(ND)]
        for xi in range(NXT):
            b2 = xi % ND
            a2 = xi % 2
            ac, ak = accd[a2], 'accd%d' % a2
            dma('sp', h1in[b2][:], H1[xi * 128:(xi + 1) * 128, :], ['H1_%d' % xi], ['h1in%d' % b2])
            for k in range(4):
                S.dma('pool', lambda e, xi=xi, k=k, b2=b2: e.indirect_dma_start(
                    out=yk[b2][k][:], out_offset=None, in_=YS[0:E * C, :],
                    in_offset=bass.IndirectOffsetOnAxis(ap=slots_all[:, xi, k:k + 1], axis=0)),
                    ys_tokens + ['slots%d' % xi], ['yk%d_%d' % (b2, k)])
            act(ac[:], yk[b2][0][:], AF.Copy, ['yk%d_0' % b2, 'gates%d' % xi], [ak], scale=gates_all[:, xi, 0:1])
            for k in range(1, 4):
                stt(ac[:], yk[b2][k][:], gates_all[:, xi, k:k + 1], ac[:], ALU.mult, ALU.add, ['yk%d_%d' % (b2, k), 'gates%d' % xi, ak], [ak])
            stt(ac[:], h1in[b2][:], ALPHA, ac[:], ALU.mult, ALU.add, ['h1in%d' % b2, ak], [ak])
            layer_norm(ac[:], od[b2][:], 4, [ak], ['od%d' % b2], 'ln2_%d' % a2, lntmp4[a2][:], lnst4[a2], 'lntmp4_%d' % a2)
            dma('sp', out[xi * 128:(xi + 1) * 128, :], od[b2][:], ['od%d' % b2], ['out_%d' % xi])
        finals = [v for k, v in S.last_w.items() if k.startswith('out_') or (dbg and k.startswith(('H0_', 'ZTM_', 'ZFM_', 'H1_')))]
        S.finish(finals)
        p4.close()
    return nc


NCORES = 8
CFG = dict(NS=2, NT=33, GT=11, E=32, C=1280)


def _core_inputs(inp, seqs, NT, E, C):
    f = lambda a: np.ascontiguousarray(a, dtype=np.float32)
    x = inp['x']; meta = inp['meta']
    rows = []
    for b in seqs:
        rows += [np.zeros((112, D), np.float32), meta, x[b]]
    m = {'xin': np.concatenate(rows, 0)}
    m['w_in'] = inp['w_in'][0]
    m['cwb'] = np.concatenate([inp['conv_w'][0].reshape(4, 8, 128).transpose(2, 1, 0).reshape(128, 32),
                               inp['conv_b'][0].reshape(8, 128).T], axis=1)
    m['gate_bias'] = inp['gate_bias'][0][None]
    m['lamv'] = np.concatenate([inp['lam_q1'][0], inp['lam_k1'][0], inp['lam_q2'][0], inp['lam_k2'][0]])[None]
    m['att_norm_g'] = inp['att_norm_g'][0][None]; m['ml_norm_g'] = inp['ml_norm_g'][0][None]
    m['w_att_out'] = inp['w_att_out'][0]; m['w_ml_out'] = inp['w_ml_out'][0]; m['w_o'] = inp['w_o'][0]
    m['lnp'] = np.stack([inp['emb_ln_g'], inp['emb_ln_b'], inp['ln1_g'][0], inp['ln1_b'][0], inp['ln2_g'][0], inp['ln2_b'][0]])
    m['w_router'] = inp['w_router'][0]; m['b_router'] = inp['b_router'][0][None]
    m['w_gu'] = inp['w_gu'][0]; m['w_down'] = inp['w_down'][0]; m['b_down'] = inp['b_down'][0]
    bg = inp['b_gu'][0]
    m['bguh'] = np.concatenate([bg[:, 0::2].reshape(E, 8, 128).transpose(0, 2, 1), bg[:, 1::2].reshape(E, 8, 128).transpose(0, 2, 1)], axis=2)
    m['b_gu'] = bg
    for k, v in host_consts(NT).items():
        m['c_' + k] = v
    m['ie1'] = np.tile((np.arange(E) + 1).astype(np.float32)[None], (128, 1))
    m['eoff'] = np.tile((np.arange(E) * C + 1).astype(np.float32)[None], (128, 1))
    return {k: f(v) for k, v in m.items()}


def kernel(**inputs):
    inp = {k: np.asarray(v) for k, v in inputs.items()}
    cfg = CFG
    nc = build(cfg['NS'], cfg['NT'], cfg['GT'], cfg['E'], cfg['C'])
    shared = None
    in_maps = []
    for c in range(NCORES):
        seqs = [c * cfg['NS'] + i for i in range(cfg['NS'])]
        if shared is None:
            shared = _core_inputs(inp, seqs, cfg['NT'], cfg['E'], cfg['C'])
            in_maps.append(shared)
        else:
            m = dict(shared)
            rows = []
            for b in seqs:
                rows += [np.zeros((112, D), np.float32), inp['meta'].astype(np.float32), inp['x'][b].astype(np.float32)]
            m['xin'] = np.ascontiguousarray(np.concatenate(rows, 0))
            in_maps.append(m)
    res = run_bass_kernel_spmd(nc, in_maps, core_ids=list(range(NCORES)))
    S_ = inp['x'].shape[1]
    outs = [np.asarray(r['out']).reshape(cfg['NS'], S_, D) for r in res.results]
    return np.concatenate(outs, axis=0).astype(np.float32)
```
